# Optimizing a Trainium2 kernel written in Bass

```python
import jax, jax.numpy as jnp
from jax import lax
import numpy as np

D_MODEL = 2048
BATCH = 4
SEQ = 8192
DEPTH = 1

GLA_HEADS = 4
GLA_KEY_DIM = D_MODEL // 2
GLA_VALUE_DIM = D_MODEL
GLA_HEAD_K = GLA_KEY_DIM // GLA_HEADS
GLA_HEAD_V = GLA_VALUE_DIM // GLA_HEADS
GLA_GATE_RANK = 16
GLA_GATE_NORMALIZER = 16.0
GLA_CHUNK = 64
POOL_WINDOWS = (2, 4, 8, 16)
POOL_GROUPS = 4
POOL_DIM = D_MODEL // 2
POOL_GROUP_DIM = POOL_DIM // POOL_GROUPS
IN_SPLITS = (GLA_KEY_DIM, GLA_KEY_DIM, GLA_VALUE_DIM, GLA_VALUE_DIM, GLA_GATE_RANK, POOL_DIM, D_MODEL, D_MODEL)
IN_DIM = 2 * GLA_KEY_DIM + 2 * GLA_VALUE_DIM + GLA_GATE_RANK + POOL_DIM + 2 * D_MODEL
N_GROUPS = 8
EXPERTS_PER_GROUP = 8
N_EXPERTS = N_GROUPS * EXPERTS_PER_GROUP
TOP_K = 2
EXPERT_HIDDEN = D_MODEL // 2
MOE_BLOCK = 128
NORM_EPS = 1e-6

kernel_name = "hybrid_gla_pool_hiermoe_adaln"


def rms_norm(x, gain):
    xf = x.astype(jnp.float32)
    y = xf * lax.rsqrt(jnp.mean(xf * xf, axis=-1, keepdims=True) + NORM_EPS)
    return (y * gain.astype(jnp.float32)).astype(x.dtype)


def modulate(x, shift, scale):
    return x * (1 + scale[:, None, :]) + shift[:, None, :]


def split_cols(z, sizes):
    outs, off = [], 0
    for s in sizes:
        outs.append(z[..., off:off + s])
        off += s
    return outs


def gla_chunked(q, k, v, log_a):
    B, H, S, dk = q.shape
    dv = v.shape[-1]
    C = GLA_CHUNK
    N = S // C
    q, k, log_a = (t.reshape(B, H, N, C, dk) for t in (q, k, log_a))
    v = v.reshape(B, H, N, C, dv)
    b = jnp.cumsum(log_a, axis=3)
    b_last = b[:, :, :, -1:, :]
    q_e = q * jnp.exp(b)
    k_e = k * jnp.exp(-b)
    k_dec = k * jnp.exp(b_last - b)
    causal = jnp.tril(jnp.ones((C, C), dtype=bool))
    att = jnp.einsum('bhnid,bhnjd->bhnij', q_e, k_e)
    att = jnp.where(causal, att, 0.0)
    o_intra = jnp.einsum('bhnij,bhnjv->bhniv', att, v)
    decay = jnp.exp(b_last[:, :, :, 0, :])

    def step(state, xs):
        qn, kn, vn, dn = xs
        o = jnp.einsum('bhid,bhdv->bhiv', qn, state)
        state = dn[..., None] * state + jnp.einsum('bhid,bhiv->bhdv', kn, vn)
        return state, o

    xs = tuple(jnp.moveaxis(t, 2, 0) for t in (q_e, k_dec, v, decay))
    _, o_inter = lax.scan(step, jnp.zeros((B, H, dk, dv), jnp.float32), xs)
    o = o_intra + jnp.moveaxis(o_inter, 0, 2)
    return o.reshape(B, H, S, dv)


def gla_branch(q, k, v, g, gk_lr, w_gk2, b_gk2, gla_norm):
    B, S, _ = q.shape
    heads = lambda t, d: t.reshape(B, S, GLA_HEADS, d).transpose(0, 2, 1, 3).astype(jnp.float32)
    qh = heads(q, GLA_HEAD_K) * (GLA_HEAD_K ** -0.5)
    kh = heads(k, GLA_HEAD_K)
    vh = heads(v, GLA_HEAD_V)
    log_a = jax.nn.log_sigmoid((gk_lr @ w_gk2 + b_gk2).astype(jnp.float32)) / GLA_GATE_NORMALIZER
    o = gla_chunked(qh, kh, vh, heads(log_a, GLA_HEAD_K))
    o = o.transpose(0, 2, 1, 3)
    o = rms_norm(o, gla_norm) * jax.nn.silu(g.reshape(B, S, GLA_HEADS, GLA_HEAD_V).astype(jnp.float32))
    return o.reshape(B, S, GLA_VALUE_DIM).astype(q.dtype)


def pool_branch(p, w_pool, pool_scale):
    B, S, _ = p.shape
    pf = p.astype(jnp.float32).reshape(B, S, POOL_GROUPS, POOL_GROUP_DIM)
    cs = jnp.cumsum(pf, axis=1)
    pos = jnp.arange(S)
    outs = []
    for gi, w in enumerate(POOL_WINDOWS):
        csg = cs[:, :, gi]
        lagged = jnp.pad(csg, ((0, 0), (w, 0), (0, 0)))[:, :S]
        count = jnp.minimum(pos + 1, w).astype(jnp.float32)[None, :, None]
        outs.append((csg - lagged) / count - pf[:, :, gi])
    m = jnp.stack(outs, axis=2)
    y = jnp.einsum('bsgc,gcd->bsgd', m, w_pool.astype(jnp.float32))
    return (y.reshape(B, S, POOL_DIM) * pool_scale.astype(jnp.float32)).astype(p.dtype)


def hier_moe(u, w_rg, b_rg, w_re, b_re, w_gate, w_up, w_down):
    B, S, D = u.shape
    T = B * S
    xt = u.reshape(T, D)
    glog = (xt @ w_rg).astype(jnp.float32) + b_rg.astype(jnp.float32)
    gprob = jax.nn.softmax(glog, axis=-1)
    gsel = jnp.argmax(glog, axis=-1).astype(jnp.int32)
    pg = jnp.take_along_axis(gprob, gsel[:, None], axis=1)[:, 0]
    elog = ((xt @ w_re).astype(jnp.float32) + b_re.astype(jnp.float32)).reshape(T, N_GROUPS, EXPERTS_PER_GROUP)
    elog = jnp.take_along_axis(elog, gsel[:, None, None], axis=1)[:, 0]
    eprob = jax.nn.softmax(elog, axis=-1)
    topv, topi = lax.top_k(eprob, TOP_K)
    topv = topv / jnp.sum(topv, axis=-1, keepdims=True)
    wts = (pg[:, None] * topv).reshape(-1)
    eid = (gsel[:, None] * EXPERTS_PER_GROUP + topi).reshape(-1).astype(jnp.int32)
    tok = jnp.repeat(jnp.arange(T, dtype=jnp.int32), TOP_K)
    A = T * TOP_K
    order = jnp.argsort(eid)
    e_sorted = eid[order]
    counts = jnp.bincount(eid, length=N_EXPERTS)
    starts = jnp.cumsum(counts) - counts
    padded = (counts + MOE_BLOCK - 1) // MOE_BLOCK * MOE_BLOCK
    pends = jnp.cumsum(padded)
    pstarts = pends - padded
    dest = pstarts[e_sorted] + jnp.arange(A, dtype=jnp.int32) - starts[e_sorted]
    P = (A + MOE_BLOCK - 1) // MOE_BLOCK * MOE_BLOCK + N_EXPERTS * MOE_BLOCK
    n_blocks = P // MOE_BLOCK
    buf_tok = jnp.full((P,), T, jnp.int32).at[dest].set(tok[order])
    buf_w = jnp.zeros((P,), jnp.float32).at[dest].set(wts[order])
    blk_e = jnp.clip(jnp.searchsorted(pends, jnp.arange(n_blocks, dtype=jnp.int32) * MOE_BLOCK, side='right'),
                     0, N_EXPERTS - 1)
    x_pad = jnp.concatenate([xt, jnp.zeros((1, D), xt.dtype)], axis=0)

    def expert_block(args):
        tb, wb, e = args
        xb = x_pad[tb]
        hdn = jax.nn.silu(xb @ w_gate[e]) * (xb @ w_up[e])
        return (hdn @ w_down[e]) * wb[:, None].astype(xb.dtype)

    yb = lax.map(expert_block, (buf_tok.reshape(n_blocks, MOE_BLOCK), buf_w.reshape(n_blocks, MOE_BLOCK), blk_e))
    y = jax.ops.segment_sum(yb.reshape(P, D), buf_tok, num_segments=T + 1)[:T]
    return y.reshape(B, S, D)


def setup_inputs(seed: int = 0) -> dict:
    key = jax.random.key(seed)
    ks = jax.random.split(key, 24)
    L, D = DEPTH, D_MODEL
    nrm = lambda k, shape, s: jax.random.normal(k, shape, jnp.float32) * s
    return {
        "x": nrm(ks[0], (BATCH, SEQ, D), 1.0),
        "c": nrm(ks[1], (BATCH, D), 1.0),
        "w_ada": nrm(ks[2], (L, D, 6 * D), 0.5 * D ** -0.5),
        "b_ada": nrm(ks[3], (L, 6 * D), 0.02),
        "norm_mix": 1.0 + nrm(ks[4], (L, D), 0.02),
        "w_in": nrm(ks[5], (L, D, IN_DIM), D ** -0.5),
        "w_gk2": nrm(ks[6], (L, GLA_GATE_RANK, GLA_KEY_DIM), GLA_GATE_RANK ** -0.5),
        "b_gk2": nrm(ks[7], (L, GLA_KEY_DIM), 0.1),
        "gla_norm": 1.0 + nrm(ks[8], (L, GLA_HEAD_V), 0.02),
        "w_a": nrm(ks[9], (L, GLA_VALUE_DIM, D), GLA_VALUE_DIM ** -0.5),
        "w_pool": nrm(ks[10], (L, POOL_GROUPS, POOL_GROUP_DIM, POOL_GROUP_DIM), POOL_GROUP_DIM ** -0.5),
        "pool_scale": 1.0 + nrm(ks[11], (L, POOL_DIM), 0.1),
        "w_b": nrm(ks[12], (L, POOL_DIM, D), POOL_DIM ** -0.5),
        "w_out": nrm(ks[13], (L, D, D), D ** -0.5),
        "norm_ffn": 1.0 + nrm(ks[14], (L, D), 0.02),
        "w_rg": nrm(ks[15], (L, D, N_GROUPS), D ** -0.5),
        "b_rg": nrm(ks[16], (L, N_GROUPS), 0.01),
        "w_re": nrm(ks[17], (L, D, N_EXPERTS), D ** -0.5),
        "b_re": nrm(ks[18], (L, N_EXPERTS), 0.01),
        "w_gate": nrm(ks[19], (L, N_EXPERTS, D, EXPERT_HIDDEN), D ** -0.5),
        "w_up": nrm(ks[20], (L, N_EXPERTS, D, EXPERT_HIDDEN), D ** -0.5),
        "w_down": nrm(ks[21], (L, N_EXPERTS, EXPERT_HIDDEN, D), EXPERT_HIDDEN ** -0.5),
        "norm_final": 1.0 + nrm(ks[22], (D,), 0.02),
    }


def reference(x, c, w_ada, b_ada, norm_mix, w_in, w_gk2, b_gk2, gla_norm, w_a, w_pool, pool_scale, w_b, w_out,
              norm_ffn, w_rg, b_rg, w_re, b_re, w_gate, w_up, w_down, norm_final):
    h = x
    for l in range(DEPTH):
        mod = jax.nn.silu(c) @ w_ada[l] + b_ada[l]
        shift1, scale1, gate1, shift2, scale2, gate2 = jnp.split(mod, 6, axis=-1)
        u = modulate(rms_norm(h, norm_mix[l]), shift1, scale1)
        z = u @ w_in[l]
        q, k, v, g, gk_lr, p, ga, gb = split_cols(z, IN_SPLITS)
        y_a = gla_branch(q, k, v, g, gk_lr, w_gk2[l], b_gk2[l], gla_norm[l]) @ w_a[l]
        y_b = pool_branch(p, w_pool[l], pool_scale[l]) @ w_b[l]
        merged = jax.nn.sigmoid(ga) * y_a + jax.nn.sigmoid(gb) * y_b
        h = h + gate1[:, None, :] * (merged @ w_out[l])
        u2 = modulate(rms_norm(h, norm_ffn[l]), shift2, scale2)
        h = h + gate2[:, None, :] * hier_moe(u2, w_rg[l], b_rg[l], w_re[l], b_re[l], w_gate[l], w_up[l], w_down[l])
    return rms_norm(h, norm_final)
```

```python
import contextlib
import numpy as np
import concourse.bass as bass
import concourse.mybir as mybir
from concourse.bass_utils import run_bass_kernel_spmd

F32 = mybir.dt.float32
BF16 = mybir.dt.bfloat16
I32 = mybir.dt.int32
AF = mybir.ActivationFunctionType
ALU = mybir.AluOpType
AX = mybir.AxisListType

D = 2048
KC = 16
IN_DIM = 11280
OQ, OK_, OV, OG, OGK, OP, OGA, OGB = 0, 1024, 2048, 4096, 6144, 6160, 7184, 9232
NE = 64
EPS = 1e-6
ENGS = ("pe", "act", "dve", "pool", "sp")


class Res:
    __slots__ = ("name", "w", "r")

    def __init__(self, name=""):
        self.name = name
        self.w = None
        self.r = []


class Op:
    __slots__ = ("eng", "fn", "seq", "waits", "needs_inc", "dma_key", "dma_val", "val")

    def __init__(self, eng, fn, seq, dma_key=None):
        self.eng = eng
        self.fn = fn
        self.seq = seq
        self.waits = []
        self.needs_inc = False
        self.dma_key = dma_key
        self.dma_val = None
        self.val = None


class Sched:
    def __init__(self, nc):
        self.nc = nc
        self.q = {e: [] for e in ENGS}
        self.dma_cnt = {}
        self.dma_last = {}
        self.pending = {e: [] for e in ENGS}

    def _dep(self, op, prod):
        if prod is None or prod is op:
            return
        if prod.dma_key is None:
            if prod.eng == op.eng and op.dma_key is None:
                if op.eng == "pe":
                    return
                if op.seq - prod.seq >= 3:
                    return
            prod.needs_inc = True
        op.waits.append(prod)

    def op(self, eng, fn, reads=(), writes=(), dma_key=None):
        o = Op(eng, fn, len(self.q[eng]), dma_key)
        if dma_key is not None:
            self.dma_cnt[dma_key] = self.dma_cnt.get(dma_key, 0) + 16
            o.dma_val = self.dma_cnt[dma_key]
            self.dma_last[dma_key] = o
        for p in self.pending[eng]:
            if p.dma_key is None:
                p.needs_inc = True
            o.waits.append(p)
        self.pending[eng] = []
        for r in reads:
            self._dep(o, r.w)
        for w in writes:
            self._dep(o, w.w)
            for rd in w.r:
                if rd.eng == eng and rd.dma_key is None and dma_key is None:
                    continue
                self._dep(o, rd)
        for r in reads:
            r.r.append(o)
        for w in writes:
            w.w = o
            w.r = []
        self.q[eng].append(o)
        return o

    def barrier(self):
        lasts = []
        for e in ENGS:
            for o in reversed(self.q[e]):
                if o.dma_key is None:
                    lasts.append(o)
                    break
        lasts += list(self.dma_last.values())
        for e in ENGS:
            self.pending[e] = self.pending[e] + [o for o in lasts]

    def emit(self, final_waits=()):
        nc = self.nc
        for e in ENGS:
            c = 0
            for o in self.q[e]:
                if o.dma_key is None and o.needs_inc:
                    c += 1
                    o.val = c
        with contextlib.ExitStack() as st:
            esem = {e: st.enter_context(nc.semaphore("p_" + e)) for e in ENGS}
            dsem = {k: st.enter_context(nc.semaphore("d_%s" % (k,))) for k in self.dma_cnt}
            block = st.enter_context(nc.Block())

            def tok(o):
                if o.dma_key is not None:
                    return dsem[o.dma_key], o.dma_val, ("d", o.dma_key)
                return esem[o.eng], o.val, ("e", o.eng)

            def run(e, eng):
                seen = {}
                for o in self.q[e]:
                    for p in o.waits:
                        s, v, k = tok(p)
                        if k == ("e", e) and o.dma_key is None and e == "pe":
                            continue
                        if seen.get(k, 0) >= v:
                            continue
                        seen[k] = v
                        eng.wait_ge(s, v)
                    ins = o.fn(eng)
                    if o.dma_key is not None:
                        ins.then_inc(dsem[o.dma_key], 16)
                    elif o.needs_inc:
                        ins.then_inc(esem[e], 1)
                if e == "sp":
                    for p in final_waits:
                        s, v, k = tok(p)
                        eng.wait_ge(s, v)

            @block.tensor
            def _(eng):
                run("pe", eng)

            @block.scalar
            def _(eng):
                run("act", eng)

            @block.vector
            def _(eng):
                run("dve", eng)

            @block.gpsimd
            def _(eng):
                run("pool", eng)

            @block.sync
            def _(eng):
                run("sp", eng)


class Buf:
    __slots__ = ("ap", "res")

    def __init__(self, ap, res=None):
        self.ap = ap
        self.res = res if res is not None else Res()

    def __getitem__(self, k):
        return Buf(self.ap[k], self.res)

    def v(self, ap):
        return Buf(ap, self.res)


_DT_SIZE = {F32: 4, BF16: 2, I32: 4}


def build_nc(NST, dbg=False):
    T = NST * 512
    NT = NST * 4
    NB = (2 * T) // 128 + NE
    PR = NB * 128
    nc = bass.Bass("TRN2", target_bir_lowering=False)
    S = Sched(nc)

    def dram(name, shape, dt, kind="ExternalInput"):
        return Buf(nc.dram_tensor(name, list(shape), dt, kind=kind).ap())

    x_cur = dram("x_cur", [T, D], F32)
    x_prev = dram("x_prev", [T, D], F32)
    cT_d = dram("cT", [128, KC], F32)
    flag_d = dram("flag", [128, 1], F32)
    w_ada = dram("w_ada", [D, 6 * D], F32)
    b_ada = dram("b_ada", [1, 6 * D], F32)
    nmixT_d = dram("nmixT", [128, KC], F32)
    w_in = dram("w_in", [D, IN_DIM], F32)
    w_gk2_d = dram("w_gk2", [16, 1024], F32)
    b_gk2_d = dram("b_gk2", [1, 1024], F32)
    gnorm_d = dram("gla_norm", [1, 512], F32)
    w_a = dram("w_a", [D, D], F32)
    w_pool_d = dram("w_pool", [4, 256, 256], F32)
    pscT_d = dram("pscT", [128, 8], F32)
    w_b = dram("w_b", [1024, D], F32)
    w_out = dram("w_out", [D, D], F32)
    nffnT_d = dram("nffnT", [128, KC], F32)
    w_r_d = dram("w_r", [D, 72], F32)
    b_r_d = dram("b_r", [1, 72], F32)
    wg_r = dram("wg_r", [NE * 4 * 128, 4096], F32)
    wu_r = dram("wu_r", [NE * 4 * 128, 4096], F32)
    wd_r = dram("wd_r", [NE * 4 * 128, 4096], F32)
    nfin_d = dram("nfin", [1, D], F32)
    cm_d = dram("cmat", [8, 128, 128], F32)
    apool_d = dram("apool", [12, 128, 128], F32)
    iota_d = dram("iotas", [128, 64 + 1 + 8], F32)
    out_d = dram("out", [T, D], F32, kind="ExternalOutput")
    h1buf = dram("h1buf", [T, D], F32, kind="Internal")
    hnbuf = dram("hnbuf", [T, D], BF16, kind="Internal")
    xg = dram("xg", [PR, D], BF16, kind="Internal")
    yslot = dram("yslot", [PR, D], F32, kind="Internal")
    dbg_d = dram("dbg", [128, 2048], F32, kind="ExternalOutput") if dbg else None

    arena = {"off": 16512, "n": 0}
    LIMIT = 229344

    def sb(shape, dt, name=None, parts=128, res=None, at=None):
        nbytes = int(np.prod(shape)) * _DT_SIZE[dt]
        nbytes = (nbytes + 63) // 64 * 64
        if at is None:
            off = arena["off"]
            arena["off"] += nbytes
            assert arena["off"] <= LIMIT, ("SBUF overflow", name, arena["off"])
        else:
            off = at
        arena["n"] += 1
        nm = "%s_%d" % (name or "t", arena["n"])
        h = nc.alloc_sbuf_tensor_at(nm, [parts] + list(shape), dt, offset=off)
        b = Buf(h.ap(), res)
        return b, off

    def sbt(shape, dt, name=None, parts=128):
        return sb(shape, dt, name, parts)[0]

    pbank = [Buf(nc.alloc_psum_tensor("pb%d" % i, [128, 512], F32).ap()) for i in range(8)]
    prot = {"f": 0, "b": 0}

    def psf():
        b = pbank[prot["f"] % 6]
        prot["f"] += 1
        return b

    def psb():
        b = pbank[6 + prot["b"] % 2]
        prot["b"] += 1
        return Buf(b.ap.bitcast(BF16), b.res)

    def _sc(v, reads, kw, name):
        if v is None:
            return
        if isinstance(v, Buf):
            kw[name] = v.ap
            reads.append(v.res)
        else:
            kw[name] = v

    def ACT(out, in_, func, bias=None, scale=None, accum=None):
        reads, kw = [in_.res], {}
        _sc(bias, reads, kw, "bias")
        _sc(scale, reads, kw, "scale")
        writes = [out.res]
        if accum is not None:
            kw["accum_out"] = accum.ap
            writes.append(accum.res)
        return S.op("act", lambda e: e.activation(out=out.ap, in_=in_.ap, func=func, **kw), reads, writes)

    def TT(eng, out, a, b, op):
        return S.op(eng, lambda e: e.tensor_tensor(out=out.ap, in0=a.ap, in1=b.ap, op=op), [a.res, b.res], [out.res])

    def TS(eng, out, a, s1, s2, op0, op1=None, accum=None):
        reads = [a.res]
        kw = {}
        s1v = s1.ap if isinstance(s1, Buf) else s1
        s2v = s2.ap if isinstance(s2, Buf) else s2
        if isinstance(s1, Buf):
            reads.append(s1.res)
        if isinstance(s2, Buf):
            reads.append(s2.res)
        writes = [out.res]
        if op1 is not None:
            kw["op1"] = op1
        if accum is not None:
            kw["accum_out"] = accum.ap
            writes.append(accum.res)
        return S.op(eng, lambda e: e.tensor_scalar(out=out.ap, in0=a.ap, scalar1=s1v, scalar2=s2v, op0=op0, **kw), reads, writes)

    def STT(eng, out, a, s, b, op0, op1):
        reads = [a.res, b.res]
        sv = s.ap if isinstance(s, Buf) else s
        if isinstance(s, Buf):
            reads.append(s.res)
        return S.op(eng, lambda e: e.scalar_tensor_tensor(out=out.ap, in0=a.ap, scalar=sv, in1=b.ap, op0=op0, op1=op1), reads, [out.res])

    def RED(eng, out, a, op, axis=AX.X):
        return S.op(eng, lambda e: e.tensor_reduce(out=out.ap, in_=a.ap, axis=axis, op=op), [a.res], [out.res])

    def CP(eng, out, a):
        return S.op(eng, lambda e: e.tensor_copy(out=out.ap, in_=a.ap), [a.res], [out.res])

    def MSET(eng, out, val):
        return S.op(eng, lambda e: e.memset(out.ap, val), [], [out.res])

    def MM(out, lhsT, rhs, start, stop):
        return S.op("pe", lambda e: e.matmul(out.ap, lhsT.ap, rhs.ap, start=start, stop=stop), [lhsT.res, rhs.res], [out.res])

    def TR(out, in_, ident):
        return S.op("pe", lambda e: e.transpose(out.ap, in_.ap, ident.ap), [in_.res, ident.res], [out.res])

    def DMA(q, key, out, in_, track_out=True):
        return S.op(q, lambda e: e.dma_start(out=out.ap, in_=in_.ap), [in_.res], [out.res] if track_out else [],
                    dma_key=key)

    bound_regs = {}

    def GATHER(key, out, src, idx, bound=None):
        def fn(e):
            kw = {}
            if bound is not None:
                if bound not in bound_regs:
                    bound_regs[bound] = e.to_reg(bound)
                kw = dict(bounds_check=bound_regs[bound], oob_is_err=False)
            return e.indirect_dma_start(out=out.ap, out_offset=None, in_=src.ap,
                                        in_offset=bass.IndirectOffsetOnAxis(ap=idx.ap, axis=0), **kw)
        return S.op("pool", fn, [src.res, idx.res], [out.res], dma_key=key)

    def SCATTER(key, dst, src, idx):
        def fn(e):
            return e.indirect_dma_start(out=dst.ap, out_offset=bass.IndirectOffsetOnAxis(ap=idx.ap, axis=0),
                                        in_=src.ap, in_offset=None)
        return S.op("pool", fn, [src.res, idx.res], [], dma_key=key)

    def bc(buf, shape):
        return Buf(buf.ap.to_broadcast(list(shape)), buf.res)

    cm32 = sbt([5, 128], F32, "cm32")
    identb = sbt([128], BF16, "identb")
    Lb = sbt([128], BF16, "Lb")
    onesb = sbt([128], BF16, "onesb")
    apb = sbt([12, 128], BF16, "apb")
    iot = sbt([73], F32, "iot")
    flag = sbt([1], F32, "flag")
    g1T = sbt([KC], F32, "g1T")
    sh1T = sbt([KC], F32, "sh1T")
    g2T = sbt([KC], F32, "g2T")
    sh2T = sbt([KC], F32, "sh2T")
    E1 = sbt([NT], F32, "E1")
    E2 = sbt([NT], F32, "E2")
    W1 = sbt([NT], F32, "W1")
    W2 = sbt([NT], F32, "W2")
    POS1 = sbt([NT], F32, "POS1")
    POS2 = sbt([NT], F32, "POS2")
    cnt_bc = sbt([NE], F32, "cnt")
    DEST1 = sbt([NT], I32, "DEST1")
    DEST2 = sbt([NT], I32, "DEST2")
    WIDX = sbt([NB * 4], I32, "WIDX")
    ident32 = cm32[:, 0, :]
    triS = cm32[:, 1, :]
    maskatt = cm32[:, 2, :]
    ones32 = cm32[:, 4, :]
    iota_e = iot[:, 0:64]
    iota_p = iot[:, 64:65]
    iota8 = iot[:, 65:73]
    phase_base = arena["off"]

    DMA("sp", "u0", cm32, Buf(cm_d.ap[0:5].rearrange("c p f -> p c f"), cm_d.res))
    DMA("sp", "u1", iot, iota_d)
    DMA("sp", "u2", flag, flag_d)
    CP("dve", identb, cm32[:, 0, :])
    CP("dve", Lb, cm32[:, 3, :])
    CP("dve", onesb, cm32[:, 4, :])
    MSET("dve", cnt_bc, 0.0)

    gate1_bc = sbt([D], F32, "gate1bc")
    bgk_bc = sbt([1024], F32, "bgkbc")
    gn_bc = sbt([512], F32, "gnbc")
    br_bc = sbt([72], F32, "brbc")
    pscT = sbt([8], F32, "pscT")
    wgk2b = sbt([1024], BF16, "wgk2b", parts=16)
    wpoolb = sbt([4, 2, 256], BF16, "wpoolb")
    wrb = sbt([KC, 72], BF16, "wrb")
    NWS = 3
    wst = [sbt([KC, 256], BF16, "wst%d" % i) for i in range(NWS)]
    uT, uT_off = sb([KC, 512], BF16, "uT")
    oT, oT_off = sb([KC, 512], BF16, "oT")
    mergedT, mg_off = sb([KC, 512], BF16, "mergedT")
    xbuf = [sb([D], F32, "xb%d" % i, res=mergedT.res, at=mg_off + i * 8192)[0] for i in range(2)]
    xh = [sb([D], F32, "xh%d" % i, res=(uT.res if i < 2 else oT.res),
             at=(uT_off if i < 2 else oT_off) + (i % 2) * 8192)[0] for i in range(4)]
    xn = sbt([D], BF16, "xn")
    gkT = sbt([512], BF16, "gkT", parts=16)
    lbuf = [sbt([256], F32, "l%d" % i) for i in range(4)]
    Epos = [sbt([512], F32, "Epos%d" % i) for i in range(2)]
    Eneg = [sbt([512], F32, "Eneg%d" % i) for i in range(2)]
    qeT = sbt([2, 512], BF16, "qeT")
    keT = sbt([2, 512], BF16, "keT")
    ke_tm = [sbt([256], BF16, "ketm%d" % i) for i in range(4)]
    vh = [sbt([512], BF16, "vh%d" % i) for i in range(4)]
    gsil = [sbt([512], BF16, "gsil%d" % i) for i in range(4)]
    gs32 = sbt([256], F32, "gs32")
    S32 = [[sbt([512], F32, "S32_%d_%d" % (h, c)) for c in range(2)] for h in range(4)]
    Sbf = [[sbt([512], BF16, "Sbf_%d_%d" % (h, c)) for c in range(2)] for h in range(4)]
    stmp = [sbt([512], F32, "stmp%d" % i) for i in range(2)]
    attb = [sbt([128], BF16, "attb%d" % i) for i in range(4)]
    og = [sbt([512], BF16, "og%d" % i) for i in range(2)]
    pg_ = [sbt([256], BF16, "pg%d" % i) for i in range(4)]
    halo = [sbt([256], BF16, "halo%d" % i) for i in range(4)]
    mT = sbt([2, 512], BF16, "mT")
    ybT = sbt([8, 512], BF16, "ybT")
    sa = [sbt([512], F32, "sa%d" % i) for i in range(2)]
    sbb = [sbt([512], F32, "sb%d" % i) for i in range(2)]
    htmp = sbt([256], F32, "htmp")
    hn = sbt([D], BF16, "hn")
    u2T = sbt([KC, 128], BF16, "u2T")
    ss = sbt([8], F32, "ss")
    rt = sbt([512], F32, "rt")
    Mb = sbt([NE], BF16, "Mb")
    modrow = sbt([256], F32, "modrow", parts=1)
    badab = sbt([256], F32, "badab")
    csT = sbt([KC], F32, "csT")
    csb = sbt([KC, 128], BF16, "csb")
    modT = sbt([4, KC], F32, "modT")
    nrmT = sbt([2, KC], F32, "nrmT")
    print("phase A SBUF bytes/partition:", arena["off"])

    DMA("sp", "u3", bgk_bc, bc(b_gk2_d, [128, 1024]))
    DMA("sp", "u4", gn_bc, bc(gnorm_d, [128, 512]))
    DMA("sp", "u5", br_bc, bc(b_r_d, [128, 72]))
    DMA("sp", "u6", pscT, pscT_d)
    DMA("sp", "u7", nrmT[:, 0, :], nmixT_d)
    DMA("sp", "u8", nrmT[:, 1, :], nffnT_d)
    DMA("sp", "u9", csT, cT_d)
    DMA("pool", "u10", wgk2b, w_gk2_d)
    DMA("pool", "u11", wpoolb, Buf(w_pool_d.ap.rearrange("g (c p) d -> p g c d", p=128), w_pool_d.res))
    DMA("pool", "u12", wrb, Buf(w_r_d.ap.rearrange("(c p) f -> p c f", p=128), w_r_d.res))
    DMA("pool", "u13", apb, Buf(apool_d.ap.rearrange("c p f -> p c f"), apool_d.res))
    for g in range(4):
        MSET("dve", halo[g], 0.0)
    for h in range(4):
        for c in range(2):
            MSET("dve", S32[h][c], 0.0)
            MSET("dve", Sbf[h][c], 0.0)

    wq = []
    wctr = {"n": 0}

    def wblock(src, row_chunks, col0, ncols, consumer):
        def load(slot):
            dst = wst[slot][:, 0:row_chunks, 0:ncols]
            sv = Buf(src.ap[0:row_chunks * 128, col0:col0 + ncols].rearrange("(c p) f -> p c f", p=128), src.res)
            DMA("pool", "ws%d" % slot, dst, sv)
        wq.append((load, consumer))

    def run_wq():
        n = len(wq)
        base = wctr["n"]
        for j in range(min(NWS - 1, n)):
            wq[j][0]((base + j) % NWS)
        for j in range(n):
            if j + NWS - 1 < n:
                wq[j + NWS - 1][0]((base + j + NWS - 1) % NWS)
            wq[j][1](wst[(base + j) % NWS])
        wctr["n"] = base + n
        del wq[:]

    ACT(csT, csT, AF.Silu)
    for ch in range(KC):
        CP("dve", csb[:, ch, :], bc(csT[:, ch:ch + 1], [128, 128]))

    def mod_block(col0, handler):
        def cons(wb):
            ps = psf()
            for ch in range(KC):
                MM(ps[:, 0:256], csb[:, ch, :], wb[:, ch, 0:256], ch == 0, ch == KC - 1)
            DMA("sp", "bada", badab, bc(b_ada[:, col0:col0 + 256], [128, 256]))
            handler(ps)
        wblock(w_ada, KC, col0, 256, cons)

    def mod_to_cols(which, j):
        def handler(ps):
            TT("dve", modrow, ps[0:1, 0:256], badab[0:1, :], ALU.add)
            for q in range(2):
                p2 = psf()
                MM(p2[:, 0:1], modrow[0:1, q * 128:(q + 1) * 128], ones32[0:1, 0:1], True, True)
                CP("dve", modT[:, which, 2 * j + q:2 * j + q + 1], p2[:, 0:1])
        return handler

    def mod_to_bc(dst, j):
        def handler(ps):
            TT("dve", dst[:, j * 256:(j + 1) * 256], ps[:, 0:256], badab, ALU.add)
        return handler

    for j in range(8):
        mod_block(0 * D + j * 256, mod_to_cols(0, j))
    for j in range(8):
        mod_block(1 * D + j * 256, mod_to_cols(1, j))
    for j in range(8):
        mod_block(2 * D + j * 256, mod_to_bc(gate1_bc, j))
    for j in range(8):
        mod_block(3 * D + j * 256, mod_to_cols(2, j))
    for j in range(8):
        mod_block(4 * D + j * 256, mod_to_cols(3, j))
    run_wq()
    CP("dve", sh1T, modT[:, 0, :])
    CP("dve", sh2T, modT[:, 2, :])
    STT("dve", g1T, modT[:, 1, :], 1.0, nrmT[:, 0, :], ALU.add, ALU.mult)
    STT("dve", g2T, modT[:, 3, :], 1.0, nrmT[:, 1, :], ALU.add, ALU.mult)

    def norm_tiles(xsrc, s):
        for i in range(4):
            xt = xbuf[i % 2]
            r0 = (s * 4 + i) * 128
            DMA("sp", "xb%d" % (i % 2), xt, xsrc[r0:r0 + 128, :])
            ACT(xn, xt, AF.Square, accum=ss[:, 0:1])
            ACT(ss[:, 1:2], ss[:, 0:1], AF.Ln, scale=1.0 / D, bias=EPS)
            ACT(ss[:, 1:2], ss[:, 1:2], AF.Exp, scale=-0.5)
            ACT(xn, xt, AF.Identity, scale=ss[:, 1:2])
            for half in range(2):
                pt = psb()
                for c in range(8):
                    k = half * 8 + c
                    TR(pt[:, c * 128:(c + 1) * 128], xn[:, k * 128:(k + 1) * 128], identb)
                for c in range(8):
                    k = half * 8 + c
                    ACT(uT[:, k, i * 128:(i + 1) * 128], pt[:, c * 128:(c + 1) * 128], AF.Identity,
                        bias=sh1T[:, k:k + 1], scale=g1T[:, k:k + 1])

    def gk_block():
        def cons(wb):
            ps = psf()
            for k in range(KC):
                MM(ps[0:16, :], wb[:, k, 0:16], uT[:, k, :], k == 0, k == KC - 1)
            CP("dve", gkT, ps[0:16, :])
        wblock(w_in, KC, OGK, 16, cons)

    def decay_tables(h):
        pl4 = [psf() for _ in range(4)]
        for i in range(4):
            MM(pl4[i][:, 0:256], gkT[:, i * 128:(i + 1) * 128], wgk2b[:, h * 256:(h + 1) * 256], True, True)
        for i in range(4):
            TT("dve", lbuf[i], pl4[i][:, 0:256], bgk_bc[:, h * 256:(h + 1) * 256], ALU.add)
            ACT(lbuf[i], lbuf[i], AF.Exp, scale=-1.0)
            ACT(lbuf[i], lbuf[i], AF.Ln, bias=1.0)
        pB = [psf(), psf()]
        for i in range(4):
            for dcc in range(2):
                MM(pB[dcc][:, i * 128:(i + 1) * 128], lbuf[i][:, dcc * 128:(dcc + 1) * 128], triS, True, True)
        for dcc in range(2):
            ACT(Epos[dcc], pB[dcc], AF.Exp)
            ACT(Eneg[dcc], pB[dcc], AF.Exp, scale=-1.0)

    def q_block(h):
        def cons(wb):
            for dcc in range(2):
                ps = psf()
                for k in range(KC):
                    MM(ps, wb[:, k, dcc * 128:(dcc + 1) * 128], uT[:, k, :], k == 0, k == KC - 1)
                STT("dve", qeT[:, dcc, :], ps, 0.0625, Epos[dcc], ALU.mult, ALU.mult)
        wblock(w_in, KC, OQ + h * 256, 256, cons)

    def k_block(h, pre):
        def cons(wb):
            for dcc in range(2):
                ps = psf()
                for k in range(KC):
                    MM(ps, wb[:, k, dcc * 128:(dcc + 1) * 128], uT[:, k, :], k == 0, k == KC - 1)
                if pre:
                    STT("dve", keT[:, dcc, :], ps, flag[:, 0:1], Eneg[dcc], ALU.mult, ALU.mult)
                else:
                    TT("dve", keT[:, dcc, :], ps, Eneg[dcc], ALU.mult)
            for i in range(4):
                pt = psb()
                for dcc in range(2):
                    TR(pt[:, dcc * 128:(dcc + 1) * 128], keT[:, dcc, i * 128:(i + 1) * 128], identb)
                CP("dve", ke_tm[i], pt[:, 0:256])
        wblock(w_in, KC, OK_ + h * 256, 256, cons)

    def tokmajor_block(col0, handler):
        def cons(wb):
            for pair in range(2):
                ps = psf()
                for ii in range(2):
                    i = pair * 2 + ii
                    for k in range(KC):
                        MM(ps[:, ii * 256:(ii + 1) * 256], uT[:, k, i * 128:(i + 1) * 128], wb[:, k, 0:256],
                           k == 0, k == KC - 1)
                for ii in range(2):
                    handler(pair * 2 + ii, ps[:, ii * 256:(ii + 1) * 256])
        wblock(w_in, KC, col0, 256, cons)

    def v_blocks(h):
        for j in range(2):
            def handler(i, pv, j=j):
                ACT(vh[i][:, j * 256:(j + 1) * 256], pv, AF.Identity)
            tokmajor_block(OV + h * 512 + j * 256, handler)

    def g_blocks(h):
        for j in range(2):
            def handler(i, pv, j=j):
                ACT(gs32, pv, AF.Silu)
                TT("dve", gsil[i][:, j * 256:(j + 1) * 256], gs32, gn_bc[:, j * 256:(j + 1) * 256], ALU.mult)
            tokmajor_block(OG + h * 512 + j * 256, handler)

    def state_update(h, i):
        col = i * 128 + 127
        for dcc in range(2):
            ps = psf()
            MM(ps, ke_tm[i][:, dcc * 128:(dcc + 1) * 128], vh[i], True, True)
            TT("dve", stmp[dcc], ps, S32[h][dcc], ALU.add)
            ACT(Sbf[h][dcc], stmp[dcc], AF.Identity, scale=Epos[dcc][:, col:col + 1])
            TS("dve", S32[h][dcc], stmp[dcc], Epos[dcc][:, col:col + 1], None, ALU.mult)

    def gla_head(h, pre):
        if not pre:
            for i in range(4):
                t0 = i * 128
                pa = psf()
                for dcc in range(2):
                    MM(pa[:, 0:128], keT[:, dcc, t0:t0 + 128], qeT[:, dcc, t0:t0 + 128], dcc == 0, dcc == 1)
                TT("dve", attb[i], pa[:, 0:128], maskatt, ALU.mult)
        for i in range(4):
            if pre:
                state_update(h, i)
                continue
            t0 = i * 128
            ab = attb[i]
            po = psf()
            for dcc in range(2):
                MM(po, qeT[:, dcc, t0:t0 + 128], Sbf[h][dcc], dcc == 0, False)
            MM(po, ab, vh[i], False, True)
            state_update(h, i)
            ogb = og[i % 2]
            ACT(ogb, po, AF.Square, accum=ss[:, 2:3])
            ACT(ss[:, 3:4], ss[:, 2:3], AF.Ln, scale=1.0 / 512, bias=EPS)
            ACT(ss[:, 3:4], ss[:, 3:4], AF.Exp, scale=-0.5)
            STT("dve", ogb, po, ss[:, 3:4], gsil[i], ALU.mult, ALU.mult)
            pt = psb()
            for vc in range(4):
                TR(pt[:, vc * 128:(vc + 1) * 128], ogb[:, vc * 128:(vc + 1) * 128], identb)
            CP("dve", oT[:, h * 4:(h + 1) * 4, t0:t0 + 128],
               Buf(pt.ap[:, 0:512].rearrange("p (c t) -> p c t", c=4), pt.res))

    def pool_group(g, s, only_halo=False, pre=False):
        def handler(i, pv):
            CP("dve", pg_[i], pv)
        tokmajor_block(OP + g * 256, handler)

        def rest():
            if only_halo:
                TS("dve", halo[g], pg_[3], flag[:, 0:1], None, ALU.mult)
                return
            pm = [psf(), psf()]
            for i in range(4):
                first = (s == 0 and i == 0)
                Ac = apb[:, (8 + g) if first else g, :]
                Ap = apb[:, 4 + g, :]
                prev = halo[g] if i == 0 else pg_[i - 1]
                for cc in range(2):
                    MM(pm[cc][:, i * 128:(i + 1) * 128], pg_[i][:, cc * 128:(cc + 1) * 128], Ac, True, False)
                    MM(pm[cc][:, i * 128:(i + 1) * 128], prev[:, cc * 128:(cc + 1) * 128], Ap, False, True)
            for cc in range(2):
                CP("dve", mT[:, cc, :], pm[cc])
            CP("dve", halo[g], pg_[3])
            for dch in range(2):
                py = psf()
                for cc in range(2):
                    MM(py, wpoolb[:, g, cc, dch * 128:(dch + 1) * 128], mT[:, cc, :], cc == 0, cc == 1)
                ACT(ybT[:, g * 2 + dch, :], py, AF.Identity, scale=pscT[:, g * 2 + dch:g * 2 + dch + 1])
        wq.append((lambda slot: None, lambda wb: rest()))

    def merge_pair(dp):
        def c_ga(wb):
            for j in range(2):
                ps = psf()
                for k in range(KC):
                    MM(ps, wb[:, k, j * 128:(j + 1) * 128], uT[:, k, :], k == 0, k == KC - 1)
                ACT(sa[j], ps, AF.Sigmoid)
        wblock(w_in, KC, OGA + dp * 256, 256, c_ga)

        def c_wa(wb):
            for j in range(2):
                ps = psf()
                for k in range(KC):
                    MM(ps, wb[:, k, j * 128:(j + 1) * 128], oT[:, k, :], k == 0, k == KC - 1)
                TT("dve", sa[j], sa[j], ps, ALU.mult)
        wblock(w_a, KC, dp * 256, 256, c_wa)

        def c_gb(wb):
            for j in range(2):
                ps = psf()
                for k in range(KC):
                    MM(ps, wb[:, k, j * 128:(j + 1) * 128], uT[:, k, :], k == 0, k == KC - 1)
                ACT(sbb[j], ps, AF.Sigmoid)
        wblock(w_in, KC, OGB + dp * 256, 256, c_gb)

        def c_wb(wb):
            for j in range(2):
                ps = psf()
                for k in range(8):
                    MM(ps, wb[:, k, j * 128:(j + 1) * 128], ybT[:, k, :], k == 0, k == 7)
                TT("dve", sbb[j], sbb[j], ps, ALU.mult)
                TT("dve", mergedT[:, dp * 2 + j, :], sa[j], sbb[j], ALU.add)
        wblock(w_b, 8, dp * 256, 256, c_wb)

    def out_blocks(s):
        def pre_load(slot):
            pass

        def load_x(wb):
            for i in range(4):
                r0 = (s * 4 + i) * 128
                DMA("sp", "xh%d" % i, xh[i], x_cur[r0:r0 + 128, :])
        wq.append((pre_load, load_x))
        for j in range(8):
            def cons(wb, j=j):
                for pair in range(2):
                    ps = psf()
                    for ii in range(2):
                        i = pair * 2 + ii
                        for k in range(KC):
                            MM(ps[:, ii * 256:(ii + 1) * 256], mergedT[:, k, i * 128:(i + 1) * 128], wb[:, k, 0:256],
                               k == 0, k == KC - 1)
                    for ii in range(2):
                        i = pair * 2 + ii
                        TT("dve", htmp, ps[:, ii * 256:(ii + 1) * 256], gate1_bc[:, j * 256:(j + 1) * 256], ALU.mult)
                        TT("dve", xh[i][:, j * 256:(j + 1) * 256], htmp, xh[i][:, j * 256:(j + 1) * 256], ALU.add)
            wblock(w_out, KC, j * 256, 256, cons)

    def route_tile(s, i):
        ti = s * 4 + i
        r0 = ti * 128
        h1 = xh[i]
        DMA("sp", "h1st%d" % i, h1buf[r0:r0 + 128, :], h1, track_out=False)
        ACT(hn, h1, AF.Square, accum=ss[:, 4:5])
        ACT(ss[:, 5:6], ss[:, 4:5], AF.Ln, scale=1.0 / D, bias=EPS)
        ACT(ss[:, 5:6], ss[:, 5:6], AF.Exp, scale=-0.5)
        ACT(hn, h1, AF.Identity, scale=ss[:, 5:6])
        DMA("sp", "hnst", hnbuf[r0:r0 + 128, :], hn, track_out=False)
        for half in range(2):
            pt = psb()
            for c in range(8):
                k = half * 8 + c
                TR(pt[:, c * 128:(c + 1) * 128], hn[:, k * 128:(k + 1) * 128], identb)
            for c in range(8):
                k = half * 8 + c
                ACT(u2T[:, k, :], pt[:, c * 128:(c + 1) * 128], AF.Identity, bias=sh2T[:, k:k + 1], scale=g2T[:, k:k + 1])
        pl = psf()
        for k in range(KC):
            MM(pl[:, 0:72], u2T[:, k, :], wrb[:, k, :], k == 0, k == KC - 1)
        lg = rt[:, 0:72]
        TT("dve", lg, pl[:, 0:72], br_bc, ALU.add)
        gmax = ss[:, 6:7]
        RED("dve", gmax, lg[:, 0:8], ALU.max)
        ohg = rt[:, 72:80]
        TS("dve", ohg, lg[:, 0:8], gmax, None, ALU.is_equal)
        ngmax = ss[:, 7:8]
        TS("dve", ngmax, gmax, -1.0, None, ALU.mult)
        gexp = rt[:, 80:88]
        gsum = rt[:, 88:89]
        ACT(gexp, lg[:, 0:8], AF.Exp, bias=ngmax, accum=gsum)
        pgp = rt[:, 89:90]
        S.op("dve", lambda e: e.reciprocal(out=pgp.ap, in_=gsum.ap), [gsum.res], [pgp.res])
        tmp64 = rt[:, 96:160]
        lgv = Buf(lg.ap[:, 8:72].rearrange("p (g j) -> p g j", g=8), lg.res)
        TT("dve", Buf(tmp64.ap.rearrange("p (g j) -> p g j", g=8), tmp64.res), lgv,
           Buf(ohg.ap.unsqueeze(2).to_broadcast([128, 8, 8]), ohg.res), ALU.mult)
        esel = rt[:, 160:168]
        RED("dve", esel, Buf(tmp64.ap.rearrange("p (g j) -> p j g", g=8), tmp64.res), ALU.add)
        m1 = rt[:, 168:169]
        RED("dve", m1, esel, ALU.max)
        oh1 = rt[:, 176:184]
        TS("dve", oh1, esel, m1, None, ALU.is_equal)
        esel2 = rt[:, 184:192]
        STT("dve", esel2, oh1, -1e30, esel, ALU.mult, ALU.add)
        m2 = rt[:, 169:170]
        RED("dve", m2, esel2, ALU.max)
        oh2 = rt[:, 192:200]
        TS("dve", oh2, esel2, m2, None, ALU.is_equal)
        dd = rt[:, 170:171]
        TT("dve", dd, m2, m1, ALU.subtract)
        rr = rt[:, 171:172]
        ACT(rr, dd, AF.Exp)
        den = rt[:, 172:173]
        TS("dve", den, rr, 1.0, None, ALU.add)
        S.op("dve", lambda e: e.reciprocal(out=den.ap, in_=den.ap), [den.res], [den.res])
        TT("dve", W1[:, ti:ti + 1], den, pgp, ALU.mult)
        TT("dve", rr, rr, den, ALU.mult)
        TT("dve", W2[:, ti:ti + 1], rr, pgp, ALU.mult)
        t8 = rt[:, 200:208]
        gid = rt[:, 173:174]
        TT("dve", t8, ohg, iota8, ALU.mult)
        RED("dve", gid, t8, ALU.add)
        j1 = rt[:, 174:175]
        t8b = rt[:, 208:216]
        TT("dve", t8b, oh1, iota8, ALU.mult)
        RED("dve", j1, t8b, ALU.add)
        j2 = rt[:, 175:176]
        t8c = rt[:, 216:224]
        TT("dve", t8c, oh2, iota8, ALU.mult)
        RED("dve", j2, t8c, ALU.add)
        STT("dve", E1[:, ti:ti + 1], gid, 8.0, j1, ALU.mult, ALU.add)
        STT("dve", E2[:, ti:ti + 1], gid, 8.0, j2, ALU.mult, ALU.add)
        M1 = rt[:, 224:288]
        M2 = rt[:, 288:352]
        TS("dve", M1, iota_e, E1[:, ti:ti + 1], None, ALU.is_equal)
        TS("dve", M2, iota_e, E2[:, ti:ti + 1], None, ALU.is_equal)
        TT("dve", Mb, M1, M2, ALU.add)
        pp = psf()
        MM(pp[:, 0:64], Lb, Mb, True, True)
        MM(pp[:, 64:128], onesb, Mb, True, True)
        posf = rt[:, 352:416]
        TT("dve", posf, pp[:, 0:64], cnt_bc, ALU.add)
        tq = rt[:, 416:480]
        TT("dve", tq, M1, posf, ALU.mult)
        RED("dve", POS1[:, ti:ti + 1], tq, ALU.add)
        tq2 = rt[:, 96:160]
        TT("dve", tq2, M2, posf, ALU.mult)
        RED("dve", POS2[:, ti:ti + 1], tq2, ALU.add)
        TT("dve", cnt_bc, cnt_bc, pp[:, 64:128], ALU.add)

    def supertile(s, pre):
        xsrc = x_prev if pre else x_cur
        norm_tiles(xsrc, s)
        gk_block()
        run_wq()
        for h in range(4):
            decay_tables(h)
            if not pre:
                q_block(h)
            k_block(h, pre)
            v_blocks(h)
            if not pre:
                g_blocks(h)
            run_wq()
            gla_head(h, pre)
        if pre:
            if s == NST - 1:
                for g in range(4):
                    pool_group(g, s, only_halo=True, pre=True)
                run_wq()
            return
        for g in range(4):
            pool_group(g, s)
        for dp in range(8):
            merge_pair(dp)
        out_blocks(s)
        run_wq()
        for i in range(4):
            route_tile(s, i)

    for s in range(NST):
        supertile(s, True)
    for s in range(NST):
        supertile(s, False)

    S.barrier()
    arena["off"] = phase_base
    ca = sbt([NE], F32, "ca")
    cb = sbt([NE], F32, "cb")
    padded = sbt([NE], F32, "padded")
    pends = sbt([NE], F32, "pends")
    pstart = sbt([NE], F32, "pstart")
    m64 = sbt([NE], F32, "m64")
    m64b = sbt([NE], F32, "m64b")
    d1f = sbt([NT], F32, "d1f")
    d2f = sbt([NT], F32, "d2f")
    blke = sbt([NB], F32, "blke")
    wif = sbt([NB, 4], F32, "wif")
    MSET("dve", cb, 0.0)
    for m in range((2 * T) // 128):
        STT("dve", cb, cnt_bc, float(128 * m), cb, ALU.is_gt, ALU.add)
    TS("dve", padded, cb, 128.0, None, ALU.mult)
    CP("dve", ca, padded)
    src, dst = ca, cb
    k = 1
    while k < NE:
        CP("dve", dst[:, 0:k], src[:, 0:k])
        TT("dve", dst[:, k:NE], src[:, k:NE], src[:, 0:NE - k], ALU.add)
        src, dst = dst, src
        k *= 2
    CP("dve", pends, src)
    TT("dve", pstart, pends, padded, ALU.subtract)
    for ti in range(NT):
        for (EE, PP, dd) in ((E1, POS1, d1f), (E2, POS2, d2f)):
            TS("dve", m64, iota_e, EE[:, ti:ti + 1], None, ALU.is_equal)
            TT("dve", m64b, m64, pstart, ALU.mult)
            RED("dve", dd[:, ti:ti + 1], m64b, ALU.add)
    TT("dve", d1f, d1f, POS1, ALU.add)
    TT("dve", d2f, d2f, POS2, ALU.add)
    CP("dve", DEST1, d1f)
    CP("dve", DEST2, d2f)
    for b in range(NB):
        TS("dve", m64, pends, float(128 * b), None, ALU.is_le)
        RED("dve", blke[:, b:b + 1], m64, ALU.add)
    blk2 = sbt([NB], F32, "blk2")
    same = sbt([NB], F32, "same")
    CP("dve", blk2[:, 0:1], blke[:, 0:1])
    TT("dve", same[:, 1:NB], blke[:, 1:NB], blke[:, 0:NB - 1], ALU.is_equal)
    STT("dve", blk2[:, 1:NB], same[:, 1:NB], float(NE), blke[:, 1:NB], ALU.mult, ALU.add)
    for pc in range(4):
        TS("dve", wif[:, :, pc], blk2, 512.0, float(pc * 128), ALU.mult, ALU.add)
    wif_flat = Buf(wif.ap.rearrange("p b c -> p (b c)"), wif.res)
    TS("dve", wif_flat, wif_flat, iota_p, None, ALU.add)
    CP("dve", WIDX, wif_flat)

    hsc = [sbt([D], BF16, "hsc%d" % i) for i in range(2)]
    for ti in range(NT):
        hb = hsc[ti % 2]
        DMA("sp", "hsc%d" % (ti % 2), hb, hnbuf[ti * 128:(ti + 1) * 128, :])
        SCATTER("sc%d" % (ti % 2), xg, hb, DEST1[:, ti:ti + 1])
        SCATTER("sc%d" % (ti % 2), xg, hb, DEST2[:, ti:ti + 1])

    xs = [sbt([D], BF16, "xs%d" % i) for i in range(2)]
    xT = [sbt([KC, 128], BF16, "xT%d" % i) for i in range(2)]
    NWB = 4
    wgp = [sbt([KC, 256], BF16, "wgp%d" % i) for i in range(NWB)]
    wup = [sbt([KC, 256], BF16, "wup%d" % i) for i in range(NWB)]
    wdp = [sbt([2, D], BF16, "wdp%d" % i) for i in range(NWB)]
    sg = sbt([4, 128], F32, "sg")
    hT = [sbt([2, 128], BF16, "hT%d" % i) for i in range(2)]
    yrow = [sbt([D], F32, "yrow%d" % i) for i in range(2)]
    print("phase B SBUF bytes/partition:", arena["off"])
    S.barrier()

    pieces = [(b, pc) for b in range(NB) for pc in range(4)]

    def load_piece(n):
        b, pc = pieces[n]
        slot = n % NWB
        idx = WIDX[:, b * 4 + pc:b * 4 + pc + 1]
        GATHER("wg%d" % slot, Buf(wgp[slot].ap.rearrange("p c f -> p (c f)"), wgp[slot].res), wg_r, idx, bound=NE * 512 - 1)
        GATHER("wu%d" % slot, Buf(wup[slot].ap.rearrange("p c f -> p (c f)"), wup[slot].res), wu_r, idx, bound=NE * 512 - 1)
        GATHER("wd%d" % slot, Buf(wdp[slot].ap.rearrange("p c f -> p (c f)"), wdp[slot].res), wd_r, idx, bound=NE * 512 - 1)

    ybanks = [pbank[0], pbank[1], pbank[2], pbank[3]]
    gub = [pbank[4], pbank[5]]
    NP = len(pieces)

    def stage_a(n):
        b, pc = pieces[n]
        xTb = xT[b % 2]
        if pc == 0:
            xsb = xs[b % 2]
            DMA("sp", "xs%d" % (b % 2), xsb, xg[b * 128:(b + 1) * 128, :])
            for half in range(2):
                pt = psb()
                for c in range(8):
                    k = half * 8 + c
                    TR(pt[:, c * 128:(c + 1) * 128], xsb[:, k * 128:(k + 1) * 128], identb)
                for c in range(8):
                    k = half * 8 + c
                    ACT(xTb[:, k, :], pt[:, c * 128:(c + 1) * 128], AF.Identity, bias=sh2T[:, k:k + 1], scale=g2T[:, k:k + 1])
        if n + 2 < NP:
            load_piece(n + 2)
        slot = n % NWB
        pgu = gub[n % 2]
        for fc in range(2):
            for gu, wsrc in ((0, wgp[slot]), (1, wup[slot])):
                q4 = fc * 2 + gu
                for k in range(KC):
                    MM(pgu[:, q4 * 128:(q4 + 1) * 128], wsrc[:, k, fc * 128:(fc + 1) * 128], xTb[:, k, :],
                       k == 0, k == KC - 1)

    def stage_b(n):
        pgu = gub[n % 2]
        htb = hT[n % 2]
        for fc in range(2):
            ACT(sg[:, fc, :], pgu[:, (fc * 2) * 128:(fc * 2 + 1) * 128], AF.Silu)
            TT("dve", htb[:, fc, :], sg[:, fc, :], pgu[:, (fc * 2 + 1) * 128:(fc * 2 + 2) * 128], ALU.mult)

    def stage_c(n):
        b, pc = pieces[n]
        slot = n % NWB
        htb = hT[n % 2]
        for dblk in range(4):
            for fc in range(2):
                MM(ybanks[dblk], htb[:, fc, :], wdp[slot][:, fc, dblk * 512:(dblk + 1) * 512],
                   pc == 0 and fc == 0, pc == 3 and fc == 1)
        if pc == 3:
            yr = yrow[b % 2]
            for dblk in range(4):
                if dblk % 2 == 0:
                    ACT(yr[:, dblk * 512:(dblk + 1) * 512], ybanks[dblk], AF.Identity)
                else:
                    CP("dve", yr[:, dblk * 512:(dblk + 1) * 512], ybanks[dblk])
            DMA("sp", "yst%d" % (b % 2), yslot[b * 128:(b + 1) * 128, :], yr, track_out=False)

    load_piece(0)
    load_piece(1)
    stage_a(0)
    for n in range(NP):
        if n + 1 < NP:
            stage_a(n + 1)
        stage_b(n)
        stage_c(n)

    S.barrier()
    arena["off"] = phase_base
    gate2_bc = sbt([D], F32, "gate2bc")
    nf_bc = sbt([D], F32, "nfbc")
    wstC = [sbt([KC, 256], BF16, "wstC%d" % i) for i in range(NWS)]
    badabC = sbt([256], F32, "badabC")
    csbC = sbt([KC, 128], BF16, "csbC")
    csTC = sbt([KC], F32, "csTC")
    h1c = [sbt([D], F32, "h1c%d" % i) for i in range(2)]
    y1c = [sbt([D], F32, "y1c%d" % i) for i in range(2)]
    y2c = [sbt([D], F32, "y2c%d" % i) for i in range(2)]
    acc = sbt([D], F32, "acc")
    outc = [sbt([D], F32, "outc%d" % i) for i in range(2)]
    junk = sbt([D], BF16, "junk")
    ssc = sbt([4], F32, "ssc")
    print("phase C SBUF bytes/partition:", arena["off"])
    S.barrier()
    wst[:] = wstC
    DMA("sp", "u14", nf_bc, bc(nfin_d, [128, D]))
    DMA("sp", "u15", csTC, cT_d)
    ACT(csTC, csTC, AF.Silu)
    for ch in range(KC):
        CP("dve", csbC[:, ch, :], bc(csTC[:, ch:ch + 1], [128, 128]))
    for j in range(8):
        def consC(wb, j=j):
            ps = psf()
            for ch in range(KC):
                MM(ps[:, 0:256], csbC[:, ch, :], wb[:, ch, 0:256], ch == 0, ch == KC - 1)
            DMA("sp", "bada", badabC, bc(b_ada[:, 5 * D + j * 256:5 * D + (j + 1) * 256], [128, 256]))
            TT("dve", gate2_bc[:, j * 256:(j + 1) * 256], ps[:, 0:256], badabC, ALU.add)
        wblock(w_ada, KC, 5 * D + j * 256, 256, consC)
    run_wq()
    last = []
    for ti in range(NT):
        r0 = ti * 128
        hb, y1, y2, ob = h1c[ti % 2], y1c[ti % 2], y2c[ti % 2], outc[ti % 2]
        DMA("sp", "h1c%d" % (ti % 2), hb, h1buf[r0:r0 + 128, :])
        GATHER("y1c%d" % (ti % 2), y1, yslot, DEST1[:, ti:ti + 1])
        GATHER("y2c%d" % (ti % 2), y2, yslot, DEST2[:, ti:ti + 1])
        TS("dve", acc, y1, W1[:, ti:ti + 1], None, ALU.mult)
        STT("dve", acc, y2, W2[:, ti:ti + 1], acc, ALU.mult, ALU.add)
        TT("dve", acc, acc, gate2_bc, ALU.mult)
        TT("dve", acc, acc, hb, ALU.add)
        ACT(junk, acc, AF.Square, accum=ssc[:, 0:1])
        ACT(ssc[:, 1:2], ssc[:, 0:1], AF.Ln, scale=1.0 / D, bias=EPS)
        ACT(ssc[:, 1:2], ssc[:, 1:2], AF.Exp, scale=-0.5)
        STT("dve", ob, acc, ssc[:, 1:2], nf_bc, ALU.mult, ALU.mult)
        last.append(DMA("sp", "oc%d" % (ti % 2), out_d[r0:r0 + 128, :], ob, track_out=False))
    S.emit(final_waits=last[-2:])
    return nc


def _consts():
    idx = np.arange(128)
    ident = np.eye(128, dtype=np.float32)
    causal = idx[:, None] <= idx[None, :]
    maskatt = causal.astype(np.float32)
    triS = maskatt * np.float32(-1.0 / 16.0)
    L = (idx[:, None] < idx[None, :]).astype(np.float32)
    ones = np.ones((128, 128), np.float32)
    cm = np.zeros((8, 128, 128), np.float32)
    cm[0], cm[1], cm[2], cm[3], cm[4] = ident, triS, maskatt, L, ones
    wins = (2, 4, 8, 16)
    acur = np.zeros((4, 128, 128), np.float32)
    aprev = np.zeros((4, 128, 128), np.float32)
    afirst = np.zeros((4, 128, 128), np.float32)
    s = idx[:, None]
    t = idx[None, :]
    for g, w in enumerate(wins):
        acur[g] = ((s <= t) & (s >= t - w + 1)).astype(np.float32) / w - ident
        aprev[g] = ((s - 128) >= (t - w + 1)).astype(np.float32) / w
        cnt = np.minimum(t + 1, w).astype(np.float32)
        afirst[g] = ((s <= t) & (s >= t - w + 1)).astype(np.float32) / cnt - ident
    iot = np.zeros((128, 73), np.float32)
    iot[:, 0:64] = np.arange(64)[None, :]
    iot[:, 64] = np.arange(128)
    iot[:, 65:73] = np.arange(8)[None, :]
    return cm, acur, aprev, afirst, iot


def _prep_shared(inp):
    f = lambda a: np.ascontiguousarray(np.asarray(a, dtype=np.float32))
    sh = {}
    sh["w_ada"] = f(inp["w_ada"][0])
    sh["b_ada"] = f(inp["b_ada"][0]).reshape(1, -1)
    sh["nmixT"] = f(inp["norm_mix"][0].reshape(KC, 128).T)
    sh["w_in"] = f(inp["w_in"][0])
    sh["w_gk2"] = f(inp["w_gk2"][0])
    sh["b_gk2"] = f(inp["b_gk2"][0]).reshape(1, -1)
    sh["gla_norm"] = f(inp["gla_norm"][0]).reshape(1, -1)
    sh["w_a"] = f(inp["w_a"][0])
    sh["w_pool"] = f(inp["w_pool"][0])
    sh["pscT"] = f(inp["pool_scale"][0].reshape(8, 128).T)
    sh["w_b"] = f(inp["w_b"][0])
    sh["w_out"] = f(inp["w_out"][0])
    sh["nffnT"] = f(inp["norm_ffn"][0].reshape(KC, 128).T)
    sh["w_r"] = f(np.concatenate([np.asarray(inp["w_rg"][0]), np.asarray(inp["w_re"][0])], axis=1))
    sh["b_r"] = f(np.concatenate([np.asarray(inp["b_rg"][0]), np.asarray(inp["b_re"][0])], axis=0)).reshape(1, -1)
    wg = np.asarray(inp["w_gate"][0], dtype=np.float32)
    wu = np.asarray(inp["w_up"][0], dtype=np.float32)
    wd = np.asarray(inp["w_down"][0], dtype=np.float32)
    sh["wg_r"] = np.ascontiguousarray(wg.reshape(NE, KC, 128, 4, 256).transpose(0, 3, 2, 1, 4)).reshape(NE * 512, 4096)
    sh["wu_r"] = np.ascontiguousarray(wu.reshape(NE, KC, 128, 4, 256).transpose(0, 3, 2, 1, 4)).reshape(NE * 512, 4096)
    sh["wd_r"] = np.ascontiguousarray(wd.reshape(NE, 4, 2, 128, D).transpose(0, 1, 3, 2, 4)).reshape(NE * 512, 4096)
    sh["nfin"] = f(inp["norm_final"]).reshape(1, -1)
    return sh


def run(inp, NST, dbg=False):
    x = np.asarray(inp["x"], dtype=np.float32)
    c = np.asarray(inp["c"], dtype=np.float32)
    B, SEQ, _ = x.shape
    T = NST * 512
    assert SEQ == 2 * T and B == 4
    nc = build_nc(NST, dbg)
    sh = _prep_shared(inp)
    cm, acur, aprev, afirst, iot = _consts()
    in_maps = []
    for core in range(8):
        b, half = core // 2, core % 2
        m = dict(sh)
        m["x_cur"] = np.ascontiguousarray(x[b, half * T:(half + 1) * T])
        m["x_prev"] = np.ascontiguousarray(x[b, 0:T])
        m["cT"] = np.ascontiguousarray(c[b].reshape(KC, 128).T)
        m["flag"] = np.full((128, 1), float(half), np.float32)
        m["cmat"] = cm
        m["apool"] = np.concatenate([acur, aprev, afirst if half == 0 else acur], axis=0)
        m["iotas"] = iot
        in_maps.append(m)
    res = run_bass_kernel_spmd(nc, in_maps, core_ids=list(range(8)))
    out = np.empty((B, SEQ, D), np.float32)
    for core in range(8):
        b, half = core // 2, core % 2
        out[b, half * T:(half + 1) * T] = res.results[core]["out"]
    return out, res


def kernel(**inputs):
    out, _ = run(inputs, 8)
    return out
```

```python
import contextlib
import numpy as np
import concourse.bass as bass
import concourse.mybir as mybir
from concourse.bass_utils import run_bass_kernel_spmd

F32 = mybir.dt.float32
BF16 = mybir.dt.bfloat16
I32 = mybir.dt.int32
AF = mybir.ActivationFunctionType
ALU = mybir.AluOpType
AX = mybir.AxisListType

D = 2048
KC = 16
IN_DIM = 11280
OQ, OK_, OV, OG, OGK, OP, OGA, OGB = 0, 1024, 2048, 4096, 6144, 6160, 7184, 9232
NE = 64
EPS = 1e-6
ENGS = ("pe", "act", "dve", "pool", "sp")


class Res:
    __slots__ = ("name", "w", "r")

    def __init__(self, name=""):
        self.name = name
        self.w = None
        self.r = []


class Op:
    __slots__ = ("eng", "fn", "seq", "waits", "needs_inc", "dma_key", "dma_val", "val")

    def __init__(self, eng, fn, seq, dma_key=None):
        self.eng = eng
        self.fn = fn
        self.seq = seq
        self.waits = []
        self.needs_inc = False
        self.dma_key = dma_key
        self.dma_val = None
        self.val = None


class Sched:
    def __init__(self, nc):
        self.nc = nc
        self.q = {e: [] for e in ENGS}
        self.dma_cnt = {}
        self.dma_last = {}
        self.pending = {e: [] for e in ENGS}

    def _dep(self, op, prod):
        if prod is None or prod is op:
            return
        if prod.dma_key is None:
            if prod.eng == op.eng and op.dma_key is None:
                if op.eng == "pe":
                    return
                if op.seq - prod.seq >= 3:
                    return
            prod.needs_inc = True
        op.waits.append(prod)

    def op(self, eng, fn, reads=(), writes=(), dma_key=None):
        o = Op(eng, fn, len(self.q[eng]), dma_key)
        if dma_key is not None:
            self.dma_cnt[dma_key] = self.dma_cnt.get(dma_key, 0) + 16
            o.dma_val = self.dma_cnt[dma_key]
            self.dma_last[dma_key] = o
        for p in self.pending[eng]:
            if p.dma_key is None:
                p.needs_inc = True
            o.waits.append(p)
        self.pending[eng] = []
        for r in reads:
            self._dep(o, r.w)
        for w in writes:
            self._dep(o, w.w)
            for rd in w.r:
                if rd.eng == eng and rd.dma_key is None and dma_key is None:
                    continue
                self._dep(o, rd)
        for r in reads:
            r.r.append(o)
        for w in writes:
            w.w = o
            w.r = []
        self.q[eng].append(o)
        return o

    def barrier(self):
        lasts = []
        for e in ENGS:
            for o in reversed(self.q[e]):
                if o.dma_key is None:
                    lasts.append(o)
                    break
        lasts += list(self.dma_last.values())
        for e in ENGS:
            self.pending[e] = self.pending[e] + [o for o in lasts]

    def emit(self, final_waits=()):
        nc = self.nc
        for e in ENGS:
            c = 0
            for o in self.q[e]:
                if o.dma_key is None and o.needs_inc:
                    c += 1
                    o.val = c
        with contextlib.ExitStack() as st:
            esem = {e: st.enter_context(nc.semaphore("p_" + e)) for e in ENGS}
            dsem = {k: st.enter_context(nc.semaphore("d_%s" % (k,))) for k in self.dma_cnt}
            block = st.enter_context(nc.Block())

            def tok(o):
                if o.dma_key is not None:
                    return dsem[o.dma_key], o.dma_val, ("d", o.dma_key)
                return esem[o.eng], o.val, ("e", o.eng)

            def run(e, eng):
                seen = {}
                for o in self.q[e]:
                    for p in o.waits:
                        s, v, k = tok(p)
                        if k == ("e", e) and o.dma_key is None and e == "pe":
                            continue
                        if seen.get(k, 0) >= v:
                            continue
                        seen[k] = v
                        eng.wait_ge(s, v)
                    ins = o.fn(eng)
                    if o.dma_key is not None:
                        ins.then_inc(dsem[o.dma_key], 16)
                    elif o.needs_inc:
                        ins.then_inc(esem[e], 1)
                if e == "sp":
                    for p in final_waits:
                        s, v, k = tok(p)
                        eng.wait_ge(s, v)

            @block.tensor
            def _(eng):
                run("pe", eng)

            @block.scalar
            def _(eng):
                run("act", eng)

            @block.vector
            def _(eng):
                run("dve", eng)

            @block.gpsimd
            def _(eng):
                run("pool", eng)

            @block.sync
            def _(eng):
                run("sp", eng)


class Buf:
    __slots__ = ("ap", "res")

    def __init__(self, ap, res=None):
        self.ap = ap
        self.res = res if res is not None else Res()

    def __getitem__(self, k):
        return Buf(self.ap[k], self.res)

    def v(self, ap):
        return Buf(ap, self.res)


_DT_SIZE = {F32: 4, BF16: 2, I32: 4}


def build_nc(NST, dbg=False):
    T = NST * 512
    NT = NST * 4
    NB = (2 * T) // 128 + NE
    PR = NB * 128
    nc = bass.Bass("TRN2", target_bir_lowering=False)
    S = Sched(nc)

    def dram(name, shape, dt, kind="ExternalInput"):
        return Buf(nc.dram_tensor(name, list(shape), dt, kind=kind).ap())

    x_cur = dram("x_cur", [T, D], F32)
    x_prev = dram("x_prev", [T, D], F32)
    cT_d = dram("cT", [128, KC], F32)
    flag_d = dram("flag", [128, 1], F32)
    w_ada = dram("w_ada", [D, 6 * D], F32)
    b_ada = dram("b_ada", [1, 6 * D], F32)
    nmixT_d = dram("nmixT", [128, KC], F32)
    w_in = dram("w_in", [D, IN_DIM], F32)
    w_gk2_d = dram("w_gk2", [16, 1024], F32)
    b_gk2_d = dram("b_gk2", [1, 1024], F32)
    gnorm_d = dram("gla_norm", [1, 512], F32)
    w_a = dram("w_a", [D, D], F32)
    w_pool_d = dram("w_pool", [4, 256, 256], F32)
    pscT_d = dram("pscT", [128, 8], F32)
    w_b = dram("w_b", [1024, D], F32)
    w_out = dram("w_out", [D, D], F32)
    nffnT_d = dram("nffnT", [128, KC], F32)
    w_r_d = dram("w_r", [D, 72], F32)
    b_r_d = dram("b_r", [1, 72], F32)
    wg_r = dram("wg_r", [NE * 4 * 128, 4096], F32)
    wu_r = dram("wu_r", [NE * 4 * 128, 4096], F32)
    wd_r = dram("wd_r", [NE * 4 * 128, 4096], F32)
    nfin_d = dram("nfin", [1, D], F32)
    cm_d = dram("cmat", [8, 128, 128], F32)
    apool_d = dram("apool", [12, 128, 128], F32)
    iota_d = dram("iotas", [128, 64 + 1 + 8], F32)
    out_d = dram("out", [T, D], F32, kind="ExternalOutput")
    h1buf = dram("h1buf", [T, D], F32, kind="Internal")
    hnbuf = dram("hnbuf", [T, D], BF16, kind="Internal")
    xg = dram("xg", [PR, D], BF16, kind="Internal")
    yslot = dram("yslot", [PR, D], F32, kind="Internal")
    dbg_d = dram("dbg", [128, 2048], F32, kind="ExternalOutput") if dbg else None

    arena = {"off": 16512, "n": 0}
    LIMIT = 229344

    def sb(shape, dt, name=None, parts=128, res=None, at=None):
        nbytes = int(np.prod(shape)) * _DT_SIZE[dt]
        nbytes = (nbytes + 63) // 64 * 64
        if at is None:
            off = arena["off"]
            arena["off"] += nbytes
            assert arena["off"] <= LIMIT, ("SBUF overflow", name, arena["off"])
        else:
            off = at
        arena["n"] += 1
        nm = "%s_%d" % (name or "t", arena["n"])
        h = nc.alloc_sbuf_tensor_at(nm, [parts] + list(shape), dt, offset=off)
        b = Buf(h.ap(), res)
        return b, off

    def sbt(shape, dt, name=None, parts=128):
        return sb(shape, dt, name, parts)[0]

    pbank = [Buf(nc.alloc_psum_tensor("pb%d" % i, [128, 512], F32).ap()) for i in range(8)]
    prot = {"f": 0, "b": 0}

    def psf():
        b = pbank[prot["f"] % 6]
        prot["f"] += 1
        return b

    def psb():
        b = pbank[6 + prot["b"] % 2]
        prot["b"] += 1
        return Buf(b.ap.bitcast(BF16), b.res)

    def _sc(v, reads, kw, name):
        if v is None:
            return
        if isinstance(v, Buf):
            kw[name] = v.ap
            reads.append(v.res)
        else:
            kw[name] = v

    def ACT(out, in_, func, bias=None, scale=None, accum=None):
        reads, kw = [in_.res], {}
        _sc(bias, reads, kw, "bias")
        _sc(scale, reads, kw, "scale")
        writes = [out.res]
        if accum is not None:
            kw["accum_out"] = accum.ap
            writes.append(accum.res)
        return S.op("act", lambda e: e.activation(out=out.ap, in_=in_.ap, func=func, **kw), reads, writes)

    def TT(eng, out, a, b, op):
        return S.op(eng, lambda e: e.tensor_tensor(out=out.ap, in0=a.ap, in1=b.ap, op=op), [a.res, b.res], [out.res])

    def TS(eng, out, a, s1, s2, op0, op1=None, accum=None):
        reads = [a.res]
        kw = {}
        s1v = s1.ap if isinstance(s1, Buf) else s1
        s2v = s2.ap if isinstance(s2, Buf) else s2
        if isinstance(s1, Buf):
            reads.append(s1.res)
        if isinstance(s2, Buf):
            reads.append(s2.res)
        writes = [out.res]
        if op1 is not None:
            kw["op1"] = op1
        if accum is not None:
            kw["accum_out"] = accum.ap
            writes.append(accum.res)
        return S.op(eng, lambda e: e.tensor_scalar(out=out.ap, in0=a.ap, scalar1=s1v, scalar2=s2v, op0=op0, **kw), reads, writes)

    def STT(eng, out, a, s, b, op0, op1):
        reads = [a.res, b.res]
        sv = s.ap if isinstance(s, Buf) else s
        if isinstance(s, Buf):
            reads.append(s.res)
        return S.op(eng, lambda e: e.scalar_tensor_tensor(out=out.ap, in0=a.ap, scalar=sv, in1=b.ap, op0=op0, op1=op1), reads, [out.res])

    def RED(eng, out, a, op, axis=AX.X):
        return S.op(eng, lambda e: e.tensor_reduce(out=out.ap, in_=a.ap, axis=axis, op=op), [a.res], [out.res])

    def CP(eng, out, a):
        return S.op(eng, lambda e: e.tensor_copy(out=out.ap, in_=a.ap), [a.res], [out.res])

    def MSET(eng, out, val):
        return S.op(eng, lambda e: e.memset(out.ap, val), [], [out.res])

    def MM(out, lhsT, rhs, start, stop):
        return S.op("pe", lambda e: e.matmul(out.ap, lhsT.ap, rhs.ap, start=start, stop=stop), [lhsT.res, rhs.res], [out.res])

    def TR(out, in_, ident):
        return S.op("pe", lambda e: e.transpose(out.ap, in_.ap, ident.ap), [in_.res, ident.res], [out.res])

    def DMA(q, key, out, in_, track_out=True):
        return S.op(q, lambda e: e.dma_start(out=out.ap, in_=in_.ap), [in_.res], [out.res] if track_out else [],
                    dma_key=key)

    bound_regs = {}

    def GATHER(key, out, src, idx, bound=None):
        def fn(e):
            kw = {}
            if bound is not None:
                if bound not in bound_regs:
                    bound_regs[bound] = e.to_reg(bound)
                kw = dict(bounds_check=bound_regs[bound], oob_is_err=False)
            return e.indirect_dma_start(out=out.ap, out_offset=None, in_=src.ap,
                                        in_offset=bass.IndirectOffsetOnAxis(ap=idx.ap, axis=0), **kw)
        return S.op("pool", fn, [src.res, idx.res], [out.res], dma_key=key)

    def SCATTER(key, dst, src, idx):
        def fn(e):
            return e.indirect_dma_start(out=dst.ap, out_offset=bass.IndirectOffsetOnAxis(ap=idx.ap, axis=0),
                                        in_=src.ap, in_offset=None)
        return S.op("pool", fn, [src.res, idx.res], [], dma_key=key)

    def bc(buf, shape):
        return Buf(buf.ap.to_broadcast(list(shape)), buf.res)

    cm32 = sbt([5, 128], F32, "cm32")
    identb = sbt([128], BF16, "identb")
    Lb = sbt([128], BF16, "Lb")
    onesb = sbt([128], BF16, "onesb")
    apb = sbt([12, 128], BF16, "apb")
    iot = sbt([73], F32, "iot")
    flag = sbt([1], F32, "flag")
    g1T = sbt([KC], F32, "g1T")
    sh1T = sbt([KC], F32, "sh1T")
    g2T = sbt([KC], F32, "g2T")
    sh2T = sbt([KC], F32, "sh2T")
    E1 = sbt([NT], F32, "E1")
    E2 = sbt([NT], F32, "E2")
    W1 = sbt([NT], F32, "W1")
    W2 = sbt([NT], F32, "W2")
    POS1 = sbt([NT], F32, "POS1")
    POS2 = sbt([NT], F32, "POS2")
    cnt_bc = sbt([NE], F32, "cnt")
    DEST1 = sbt([NT], I32, "DEST1")
    DEST2 = sbt([NT], I32, "DEST2")
    WIDX = sbt([NB * 4], I32, "WIDX")
    ident32 = cm32[:, 0, :]
    triS = cm32[:, 1, :]
    maskatt = cm32[:, 2, :]
    ones32 = cm32[:, 4, :]
    iota_e = iot[:, 0:64]
    iota_p = iot[:, 64:65]
    iota8 = iot[:, 65:73]
    phase_base = arena["off"]

    DMA("sp", "u0", cm32, Buf(cm_d.ap[0:5].rearrange("c p f -> p c f"), cm_d.res))
    DMA("sp", "u1", iot, iota_d)
    DMA("sp", "u2", flag, flag_d)
    CP("dve", identb, cm32[:, 0, :])
    CP("dve", Lb, cm32[:, 3, :])
    CP("dve", onesb, cm32[:, 4, :])
    MSET("dve", cnt_bc, 0.0)

    gate1_bc = sbt([D], F32, "gate1bc")
    bgk_bc = sbt([1024], F32, "bgkbc")
    gn_bc = sbt([512], F32, "gnbc")
    br_bc = sbt([72], F32, "brbc")
    pscT = sbt([8], F32, "pscT")
    wgk2b = sbt([1024], BF16, "wgk2b", parts=16)
    wpoolb = sbt([4, 2, 256], BF16, "wpoolb")
    wrb = sbt([KC, 72], BF16, "wrb")
    NWS = 3
    wst = [sbt([KC, 256], BF16, "wst%d" % i) for i in range(NWS)]
    uT, uT_off = sb([KC, 512], BF16, "uT")
    oT, oT_off = sb([KC, 512], BF16, "oT")
    mergedT, mg_off = sb([KC, 512], BF16, "mergedT")
    xbuf = [sb([D], F32, "xb%d" % i, res=mergedT.res, at=mg_off + i * 8192)[0] for i in range(2)]
    xh = [sb([D], F32, "xh%d" % i, res=(uT.res if i < 2 else oT.res),
             at=(uT_off if i < 2 else oT_off) + (i % 2) * 8192)[0] for i in range(4)]
    xn = sbt([D], BF16, "xn")
    gkT = sbt([512], BF16, "gkT", parts=16)
    lbuf = [sbt([256], F32, "l%d" % i) for i in range(4)]
    Epos = [sbt([512], F32, "Epos%d" % i) for i in range(2)]
    Eneg = [sbt([512], F32, "Eneg%d" % i) for i in range(2)]
    qeT = sbt([2, 512], BF16, "qeT")
    keT = sbt([2, 512], BF16, "keT")
    ke_tm = [sbt([256], BF16, "ketm%d" % i) for i in range(4)]
    vh = [sbt([512], BF16, "vh%d" % i) for i in range(4)]
    gsil = [sbt([512], BF16, "gsil%d" % i) for i in range(4)]
    gs32 = sbt([256], F32, "gs32")
    S32 = [[sbt([512], F32, "S32_%d_%d" % (h, c)) for c in range(2)] for h in range(4)]
    Sbf = [[sbt([512], BF16, "Sbf_%d_%d" % (h, c)) for c in range(2)] for h in range(4)]
    stmp = [sbt([512], F32, "stmp%d" % i) for i in range(2)]
    attb = [sbt([128], BF16, "attb%d" % i) for i in range(4)]
    og = [sbt([512], BF16, "og%d" % i) for i in range(2)]
    pg_ = [sbt([256], BF16, "pg%d" % i) for i in range(4)]
    halo = [sbt([256], BF16, "halo%d" % i) for i in range(4)]
    mT = sbt([2, 512], BF16, "mT")
    ybT = sbt([8, 512], BF16, "ybT")
    sa = [sbt([512], F32, "sa%d" % i) for i in range(2)]
    sbb = [sbt([512], F32, "sb%d" % i) for i in range(2)]
    htmp = sbt([256], F32, "htmp")
    hn = sbt([D], BF16, "hn")
    u2T = sbt([KC, 128], BF16, "u2T")
    ss = sbt([8], F32, "ss")
    rt = sbt([512], F32, "rt")
    Mb = sbt([NE], BF16, "Mb")
    modrow = sbt([256], F32, "modrow", parts=1)
    badab = sbt([256], F32, "badab")
    csT = sbt([KC], F32, "csT")
    csb = sbt([KC, 128], BF16, "csb")
    modT = sbt([4, KC], F32, "modT")
    nrmT = sbt([2, KC], F32, "nrmT")
    print("phase A SBUF bytes/partition:", arena["off"])

    DMA("sp", "u3", bgk_bc, bc(b_gk2_d, [128, 1024]))
    DMA("sp", "u4", gn_bc, bc(gnorm_d, [128, 512]))
    DMA("sp", "u5", br_bc, bc(b_r_d, [128, 72]))
    DMA("sp", "u6", pscT, pscT_d)
    DMA("sp", "u7", nrmT[:, 0, :], nmixT_d)
    DMA("sp", "u8", nrmT[:, 1, :], nffnT_d)
    DMA("sp", "u9", csT, cT_d)
    DMA("pool", "u10", wgk2b, w_gk2_d)
    DMA("pool", "u11", wpoolb, Buf(w_pool_d.ap.rearrange("g (c p) d -> p g c d", p=128), w_pool_d.res))
    DMA("pool", "u12", wrb, Buf(w_r_d.ap.rearrange("(c p) f -> p c f", p=128), w_r_d.res))
    DMA("pool", "u13", apb, Buf(apool_d.ap.rearrange("c p f -> p c f"), apool_d.res))
    for g in range(4):
        MSET("dve", halo[g], 0.0)
    for h in range(4):
        for c in range(2):
            MSET("dve", S32[h][c], 0.0)
            MSET("dve", Sbf[h][c], 0.0)

    wq = []
    wctr = {"n": 0}

    def wblock(src, row_chunks, col0, ncols, consumer):
        def load(slot):
            dst = wst[slot][:, 0:row_chunks, 0:ncols]
            sv = Buf(src.ap[0:row_chunks * 128, col0:col0 + ncols].rearrange("(c p) f -> p c f", p=128), src.res)
            DMA("pool", "ws%d" % slot, dst, sv)
        wq.append((load, consumer))

    def run_wq():
        n = len(wq)
        base = wctr["n"]
        for j in range(min(NWS - 1, n)):
            wq[j][0]((base + j) % NWS)
        for j in range(n):
            if j + NWS - 1 < n:
                wq[j + NWS - 1][0]((base + j + NWS - 1) % NWS)
            wq[j][1](wst[(base + j) % NWS])
        wctr["n"] = base + n
        del wq[:]

    ACT(csT, csT, AF.Silu)
    for ch in range(KC):
        CP("dve", csb[:, ch, :], bc(csT[:, ch:ch + 1], [128, 128]))

    def mod_block(col0, handler):
        def cons(wb):
            ps = psf()
            for ch in range(KC):
                MM(ps[:, 0:256], csb[:, ch, :], wb[:, ch, 0:256], ch == 0, ch == KC - 1)
            DMA("sp", "bada", badab, bc(b_ada[:, col0:col0 + 256], [128, 256]))
            handler(ps)
        wblock(w_ada, KC, col0, 256, cons)

    def mod_to_cols(which, j):
        def handler(ps):
            TT("dve", modrow, ps[0:1, 0:256], badab[0:1, :], ALU.add)
            for q in range(2):
                p2 = psf()
                MM(p2[:, 0:1], modrow[0:1, q * 128:(q + 1) * 128], ones32[0:1, 0:1], True, True)
                CP("dve", modT[:, which, 2 * j + q:2 * j + q + 1], p2[:, 0:1])
        return handler

    def mod_to_bc(dst, j):
        def handler(ps):
            TT("dve", dst[:, j * 256:(j + 1) * 256], ps[:, 0:256], badab, ALU.add)
        return handler

    for j in range(8):
        mod_block(0 * D + j * 256, mod_to_cols(0, j))
    for j in range(8):
        mod_block(1 * D + j * 256, mod_to_cols(1, j))
    for j in range(8):
        mod_block(2 * D + j * 256, mod_to_bc(gate1_bc, j))
    for j in range(8):
        mod_block(3 * D + j * 256, mod_to_cols(2, j))
    for j in range(8):
        mod_block(4 * D + j * 256, mod_to_cols(3, j))
    run_wq()
    CP("dve", sh1T, modT[:, 0, :])
    CP("dve", sh2T, modT[:, 2, :])
    STT("dve", g1T, modT[:, 1, :], 1.0, nrmT[:, 0, :], ALU.add, ALU.mult)
    STT("dve", g2T, modT[:, 3, :], 1.0, nrmT[:, 1, :], ALU.add, ALU.mult)

    def norm_tiles(xsrc, s):
        for i in range(4):
            xt = xbuf[i % 2]
            r0 = (s * 4 + i) * 128
            DMA("sp", "xb%d" % (i % 2), xt, xsrc[r0:r0 + 128, :])
            ACT(xn, xt, AF.Square, accum=ss[:, 0:1])
            ACT(ss[:, 1:2], ss[:, 0:1], AF.Ln, scale=1.0 / D, bias=EPS)
            ACT(ss[:, 1:2], ss[:, 1:2], AF.Exp, scale=-0.5)
            ACT(xn, xt, AF.Identity, scale=ss[:, 1:2])
            for half in range(2):
                pt = psb()
                for c in range(8):
                    k = half * 8 + c
                    TR(pt[:, c * 128:(c + 1) * 128], xn[:, k * 128:(k + 1) * 128], identb)
                for c in range(8):
                    k = half * 8 + c
                    if c % 2 == 0:
                        ACT(uT[:, k, i * 128:(i + 1) * 128], pt[:, c * 128:(c + 1) * 128], AF.Identity,
                            bias=sh1T[:, k:k + 1], scale=g1T[:, k:k + 1])
                    else:
                        TS("dve", uT[:, k, i * 128:(i + 1) * 128], pt[:, c * 128:(c + 1) * 128],
                           g1T[:, k:k + 1], sh1T[:, k:k + 1], ALU.mult, ALU.add)

    def gk_block():
        def cons(wb):
            ps = psf()
            for k in range(KC):
                MM(ps[0:16, :], wb[:, k, 0:16], uT[:, k, :], k == 0, k == KC - 1)
            CP("dve", gkT, ps[0:16, :])
        wblock(w_in, KC, OGK, 16, cons)

    def decay_tables(h):
        pl4 = [psf() for _ in range(4)]
        for i in range(4):
            MM(pl4[i][:, 0:256], gkT[:, i * 128:(i + 1) * 128], wgk2b[:, h * 256:(h + 1) * 256], True, True)
        for i in range(4):
            TT("dve", lbuf[i], pl4[i][:, 0:256], bgk_bc[:, h * 256:(h + 1) * 256], ALU.add)
            ACT(lbuf[i], lbuf[i], AF.Exp, scale=-1.0)
            ACT(lbuf[i], lbuf[i], AF.Ln, bias=1.0)
        pB = [psf(), psf()]
        for i in range(4):
            for dcc in range(2):
                MM(pB[dcc][:, i * 128:(i + 1) * 128], lbuf[i][:, dcc * 128:(dcc + 1) * 128], triS, True, True)
        for dcc in range(2):
            ACT(Epos[dcc], pB[dcc], AF.Exp)
            ACT(Eneg[dcc], pB[dcc], AF.Exp, scale=-1.0)

    def q_block(h):
        def cons(wb):
            for dcc in range(2):
                ps = psf()
                for k in range(KC):
                    MM(ps, wb[:, k, dcc * 128:(dcc + 1) * 128], uT[:, k, :], k == 0, k == KC - 1)
                STT("dve", qeT[:, dcc, :], ps, 0.0625, Epos[dcc], ALU.mult, ALU.mult)
        wblock(w_in, KC, OQ + h * 256, 256, cons)

    def k_block(h, pre):
        def cons(wb):
            for dcc in range(2):
                ps = psf()
                for k in range(KC):
                    MM(ps, wb[:, k, dcc * 128:(dcc + 1) * 128], uT[:, k, :], k == 0, k == KC - 1)
                if pre:
                    STT("dve", keT[:, dcc, :], ps, flag[:, 0:1], Eneg[dcc], ALU.mult, ALU.mult)
                else:
                    TT("dve", keT[:, dcc, :], ps, Eneg[dcc], ALU.mult)
            for i in range(4):
                pt = psb()
                for dcc in range(2):
                    TR(pt[:, dcc * 128:(dcc + 1) * 128], keT[:, dcc, i * 128:(i + 1) * 128], identb)
                CP("dve", ke_tm[i], pt[:, 0:256])
        wblock(w_in, KC, OK_ + h * 256, 256, cons)

    def tokmajor_block(col0, handler):
        def cons(wb):
            for pair in range(2):
                ps = psf()
                for ii in range(2):
                    i = pair * 2 + ii
                    for k in range(KC):
                        MM(ps[:, ii * 256:(ii + 1) * 256], uT[:, k, i * 128:(i + 1) * 128], wb[:, k, 0:256],
                           k == 0, k == KC - 1)
                for ii in range(2):
                    handler(pair * 2 + ii, ps[:, ii * 256:(ii + 1) * 256])
        wblock(w_in, KC, col0, 256, cons)

    def v_blocks(h):
        for j in range(2):
            def handler(i, pv, j=j):
                ACT(vh[i][:, j * 256:(j + 1) * 256], pv, AF.Identity)
            tokmajor_block(OV + h * 512 + j * 256, handler)

    def g_blocks(h):
        for j in range(2):
            def handler(i, pv, j=j):
                ACT(gs32, pv, AF.Silu)
                TT("dve", gsil[i][:, j * 256:(j + 1) * 256], gs32, gn_bc[:, j * 256:(j + 1) * 256], ALU.mult)
            tokmajor_block(OG + h * 512 + j * 256, handler)

    def state_update(h, i):
        col = i * 128 + 127
        for dcc in range(2):
            ps = psf()
            MM(ps, ke_tm[i][:, dcc * 128:(dcc + 1) * 128], vh[i], True, True)
            TT("dve", stmp[dcc], ps, S32[h][dcc], ALU.add)
            ACT(Sbf[h][dcc], stmp[dcc], AF.Identity, scale=Epos[dcc][:, col:col + 1])
            TS("dve", S32[h][dcc], stmp[dcc], Epos[dcc][:, col:col + 1], None, ALU.mult)

    def gla_head(h, pre):
        if not pre:
            for i in range(4):
                t0 = i * 128
                pa = psf()
                for dcc in range(2):
                    MM(pa[:, 0:128], keT[:, dcc, t0:t0 + 128], qeT[:, dcc, t0:t0 + 128], dcc == 0, dcc == 1)
                TT("dve", attb[i], pa[:, 0:128], maskatt, ALU.mult)
        for i in range(4):
            if pre:
                state_update(h, i)
                continue
            t0 = i * 128
            ab = attb[i]
            po = psf()
            for dcc in range(2):
                MM(po, qeT[:, dcc, t0:t0 + 128], Sbf[h][dcc], dcc == 0, False)
            MM(po, ab, vh[i], False, True)
            state_update(h, i)
            ogb = og[i % 2]
            ACT(ogb, po, AF.Square, accum=ss[:, 2:3])
            ACT(ss[:, 3:4], ss[:, 2:3], AF.Ln, scale=1.0 / 512, bias=EPS)
            ACT(ss[:, 3:4], ss[:, 3:4], AF.Exp, scale=-0.5)
            STT("dve", ogb, po, ss[:, 3:4], gsil[i], ALU.mult, ALU.mult)
            pt = psb()
            for vc in range(4):
                TR(pt[:, vc * 128:(vc + 1) * 128], ogb[:, vc * 128:(vc + 1) * 128], identb)
            CP("dve", oT[:, h * 4:(h + 1) * 4, t0:t0 + 128],
               Buf(pt.ap[:, 0:512].rearrange("p (c t) -> p c t", c=4), pt.res))

    def pool_group(g, s, only_halo=False, pre=False):
        def handler(i, pv):
            CP("dve", pg_[i], pv)
        tokmajor_block(OP + g * 256, handler)

        def rest():
            if only_halo:
                TS("dve", halo[g], pg_[3], flag[:, 0:1], None, ALU.mult)
                return
            pm = [psf(), psf()]
            for i in range(4):
                first = (s == 0 and i == 0)
                Ac = apb[:, (8 + g) if first else g, :]
                Ap = apb[:, 4 + g, :]
                prev = halo[g] if i == 0 else pg_[i - 1]
                for cc in range(2):
                    MM(pm[cc][:, i * 128:(i + 1) * 128], pg_[i][:, cc * 128:(cc + 1) * 128], Ac, True, False)
                    MM(pm[cc][:, i * 128:(i + 1) * 128], prev[:, cc * 128:(cc + 1) * 128], Ap, False, True)
            for cc in range(2):
                CP("dve", mT[:, cc, :], pm[cc])
            CP("dve", halo[g], pg_[3])
            for dch in range(2):
                py = psf()
                for cc in range(2):
                    MM(py, wpoolb[:, g, cc, dch * 128:(dch + 1) * 128], mT[:, cc, :], cc == 0, cc == 1)
                ACT(ybT[:, g * 2 + dch, :], py, AF.Identity, scale=pscT[:, g * 2 + dch:g * 2 + dch + 1])
        wq.append((lambda slot: None, lambda wb: rest()))

    def merge_pair(dp):
        def c_ga(wb):
            for j in range(2):
                ps = psf()
                for k in range(KC):
                    MM(ps, wb[:, k, j * 128:(j + 1) * 128], uT[:, k, :], k == 0, k == KC - 1)
                ACT(sa[j], ps, AF.Sigmoid)
        wblock(w_in, KC, OGA + dp * 256, 256, c_ga)

        def c_wa(wb):
            for j in range(2):
                ps = psf()
                for k in range(KC):
                    MM(ps, wb[:, k, j * 128:(j + 1) * 128], oT[:, k, :], k == 0, k == KC - 1)
                TT("dve", sa[j], sa[j], ps, ALU.mult)
        wblock(w_a, KC, dp * 256, 256, c_wa)

        def c_gb(wb):
            for j in range(2):
                ps = psf()
                for k in range(KC):
                    MM(ps, wb[:, k, j * 128:(j + 1) * 128], uT[:, k, :], k == 0, k == KC - 1)
                ACT(sbb[j], ps, AF.Sigmoid)
        wblock(w_in, KC, OGB + dp * 256, 256, c_gb)

        def c_wb(wb):
            for j in range(2):
                ps = psf()
                for k in range(8):
                    MM(ps, wb[:, k, j * 128:(j + 1) * 128], ybT[:, k, :], k == 0, k == 7)
                TT("dve", sbb[j], sbb[j], ps, ALU.mult)
                TT("dve", mergedT[:, dp * 2 + j, :], sa[j], sbb[j], ALU.add)
        wblock(w_b, 8, dp * 256, 256, c_wb)

    def out_blocks(s):
        def pre_load(slot):
            pass

        def load_x(wb):
            for i in range(4):
                r0 = (s * 4 + i) * 128
                DMA("sp", "xh%d" % i, xh[i], x_cur[r0:r0 + 128, :])
        wq.append((pre_load, load_x))
        for j in range(8):
            def cons(wb, j=j):
                for pair in range(2):
                    ps = psf()
                    for ii in range(2):
                        i = pair * 2 + ii
                        for k in range(KC):
                            MM(ps[:, ii * 256:(ii + 1) * 256], mergedT[:, k, i * 128:(i + 1) * 128], wb[:, k, 0:256],
                               k == 0, k == KC - 1)
                    for ii in range(2):
                        i = pair * 2 + ii
                        TT("dve", htmp, ps[:, ii * 256:(ii + 1) * 256], gate1_bc[:, j * 256:(j + 1) * 256], ALU.mult)
                        TT("dve", xh[i][:, j * 256:(j + 1) * 256], htmp, xh[i][:, j * 256:(j + 1) * 256], ALU.add)
            wblock(w_out, KC, j * 256, 256, cons)

    def route_tile(s, i):
        ti = s * 4 + i
        r0 = ti * 128
        h1 = xh[i]
        DMA("sp", "h1st%d" % i, h1buf[r0:r0 + 128, :], h1, track_out=False)
        ACT(hn, h1, AF.Square, accum=ss[:, 4:5])
        ACT(ss[:, 5:6], ss[:, 4:5], AF.Ln, scale=1.0 / D, bias=EPS)
        ACT(ss[:, 5:6], ss[:, 5:6], AF.Exp, scale=-0.5)
        ACT(hn, h1, AF.Identity, scale=ss[:, 5:6])
        DMA("sp", "hnst", hnbuf[r0:r0 + 128, :], hn, track_out=False)
        for half in range(2):
            pt = psb()
            for c in range(8):
                k = half * 8 + c
                TR(pt[:, c * 128:(c + 1) * 128], hn[:, k * 128:(k + 1) * 128], identb)
            for c in range(8):
                k = half * 8 + c
                ACT(u2T[:, k, :], pt[:, c * 128:(c + 1) * 128], AF.Identity, bias=sh2T[:, k:k + 1], scale=g2T[:, k:k + 1])
        pl = psf()
        for k in range(KC):
            MM(pl[:, 0:72], u2T[:, k, :], wrb[:, k, :], k == 0, k == KC - 1)
        lg = rt[:, 0:72]
        TT("dve", lg, pl[:, 0:72], br_bc, ALU.add)
        gmax = ss[:, 6:7]
        RED("dve", gmax, lg[:, 0:8], ALU.max)
        ohg = rt[:, 72:80]
        TS("dve", ohg, lg[:, 0:8], gmax, None, ALU.is_equal)
        ngmax = ss[:, 7:8]
        TS("dve", ngmax, gmax, -1.0, None, ALU.mult)
        gexp = rt[:, 80:88]
        gsum = rt[:, 88:89]
        ACT(gexp, lg[:, 0:8], AF.Exp, bias=ngmax, accum=gsum)
        pgp = rt[:, 89:90]
        S.op("dve", lambda e: e.reciprocal(out=pgp.ap, in_=gsum.ap), [gsum.res], [pgp.res])
        tmp64 = rt[:, 96:160]
        lgv = Buf(lg.ap[:, 8:72].rearrange("p (g j) -> p g j", g=8), lg.res)
        TT("dve", Buf(tmp64.ap.rearrange("p (g j) -> p g j", g=8), tmp64.res), lgv,
           Buf(ohg.ap.unsqueeze(2).to_broadcast([128, 8, 8]), ohg.res), ALU.mult)
        esel = rt[:, 160:168]
        RED("dve", esel, Buf(tmp64.ap.rearrange("p (g j) -> p j g", g=8), tmp64.res), ALU.add)
        m1 = rt[:, 168:169]
        RED("dve", m1, esel, ALU.max)
        oh1 = rt[:, 176:184]
        TS("dve", oh1, esel, m1, None, ALU.is_equal)
        esel2 = rt[:, 184:192]
        STT("dve", esel2, oh1, -1e30, esel, ALU.mult, ALU.add)
        m2 = rt[:, 169:170]
        RED("dve", m2, esel2, ALU.max)
        oh2 = rt[:, 192:200]
        TS("dve", oh2, esel2, m2, None, ALU.is_equal)
        dd = rt[:, 170:171]
        TT("dve", dd, m2, m1, ALU.subtract)
        rr = rt[:, 171:172]
        ACT(rr, dd, AF.Exp)
        den = rt[:, 172:173]
        TS("dve", den, rr, 1.0, None, ALU.add)
        S.op("dve", lambda e: e.reciprocal(out=den.ap, in_=den.ap), [den.res], [den.res])
        TT("dve", W1[:, ti:ti + 1], den, pgp, ALU.mult)
        TT("dve", rr, rr, den, ALU.mult)
        TT("dve", W2[:, ti:ti + 1], rr, pgp, ALU.mult)
        t8 = rt[:, 200:208]
        gid = rt[:, 173:174]
        TT("dve", t8, ohg, iota8, ALU.mult)
        RED("dve", gid, t8, ALU.add)
        j1 = rt[:, 174:175]
        t8b = rt[:, 208:216]
        TT("dve", t8b, oh1, iota8, ALU.mult)
        RED("dve", j1, t8b, ALU.add)
        j2 = rt[:, 175:176]
        t8c = rt[:, 216:224]
        TT("dve", t8c, oh2, iota8, ALU.mult)
        RED("dve", j2, t8c, ALU.add)
        STT("dve", E1[:, ti:ti + 1], gid, 8.0, j1, ALU.mult, ALU.add)
        STT("dve", E2[:, ti:ti + 1], gid, 8.0, j2, ALU.mult, ALU.add)
        M1 = rt[:, 224:288]
        M2 = rt[:, 288:352]
        TS("dve", M1, iota_e, E1[:, ti:ti + 1], None, ALU.is_equal)
        TS("dve", M2, iota_e, E2[:, ti:ti + 1], None, ALU.is_equal)
        TT("dve", Mb, M1, M2, ALU.add)
        pp = psf()
        MM(pp[:, 0:64], Lb, Mb, True, True)
        MM(pp[:, 64:128], onesb, Mb, True, True)
        posf = rt[:, 352:416]
        TT("dve", posf, pp[:, 0:64], cnt_bc, ALU.add)
        tq = rt[:, 416:480]
        TT("dve", tq, M1, posf, ALU.mult)
        RED("dve", POS1[:, ti:ti + 1], tq, ALU.add)
        tq2 = rt[:, 96:160]
        TT("dve", tq2, M2, posf, ALU.mult)
        RED("dve", POS2[:, ti:ti + 1], tq2, ALU.add)
        TT("dve", cnt_bc, cnt_bc, pp[:, 64:128], ALU.add)

    def supertile(s, pre):
        xsrc = x_prev if pre else x_cur
        norm_tiles(xsrc, s)
        gk_block()
        run_wq()
        for h in range(4):
            decay_tables(h)
            if not pre:
                q_block(h)
            k_block(h, pre)
            v_blocks(h)
            if not pre:
                g_blocks(h)
            run_wq()
            gla_head(h, pre)
        if pre:
            if s == NST - 1:
                for g in range(4):
                    pool_group(g, s, only_halo=True, pre=True)
                run_wq()
            return
        for g in range(4):
            pool_group(g, s)
        for dp in range(8):
            merge_pair(dp)
        out_blocks(s)
        run_wq()
        for i in range(4):
            route_tile(s, i)

    for s in range(NST):
        supertile(s, True)
    for s in range(NST):
        supertile(s, False)

    S.barrier()
    arena["off"] = phase_base
    ca = sbt([NE], F32, "ca")
    cb = sbt([NE], F32, "cb")
    padded = sbt([NE], F32, "padded")
    pends = sbt([NE], F32, "pends")
    pstart = sbt([NE], F32, "pstart")
    m64 = sbt([NE], F32, "m64")
    m64b = sbt([NE], F32, "m64b")
    d1f = sbt([NT], F32, "d1f")
    d2f = sbt([NT], F32, "d2f")
    blke = sbt([NB], F32, "blke")
    wif = sbt([NB, 4], F32, "wif")
    MSET("dve", cb, 0.0)
    for m in range((2 * T) // 128):
        STT("dve", cb, cnt_bc, float(128 * m), cb, ALU.is_gt, ALU.add)
    TS("dve", padded, cb, 128.0, None, ALU.mult)
    CP("dve", ca, padded)
    src, dst = ca, cb
    k = 1
    while k < NE:
        CP("dve", dst[:, 0:k], src[:, 0:k])
        TT("dve", dst[:, k:NE], src[:, k:NE], src[:, 0:NE - k], ALU.add)
        src, dst = dst, src
        k *= 2
    CP("dve", pends, src)
    TT("dve", pstart, pends, padded, ALU.subtract)
    for ti in range(NT):
        for (EE, PP, dd) in ((E1, POS1, d1f), (E2, POS2, d2f)):
            TS("dve", m64, iota_e, EE[:, ti:ti + 1], None, ALU.is_equal)
            TT("dve", m64b, m64, pstart, ALU.mult)
            RED("dve", dd[:, ti:ti + 1], m64b, ALU.add)
    TT("dve", d1f, d1f, POS1, ALU.add)
    TT("dve", d2f, d2f, POS2, ALU.add)
    CP("dve", DEST1, d1f)
    CP("dve", DEST2, d2f)
    for b in range(NB):
        TS("dve", m64, pends, float(128 * b), None, ALU.is_le)
        RED("dve", blke[:, b:b + 1], m64, ALU.add)
    blk2 = sbt([NB], F32, "blk2")
    same = sbt([NB], F32, "same")
    CP("dve", blk2[:, 0:1], blke[:, 0:1])
    TT("dve", same[:, 1:NB], blke[:, 1:NB], blke[:, 0:NB - 1], ALU.is_equal)
    STT("dve", blk2[:, 1:NB], same[:, 1:NB], float(NE), blke[:, 1:NB], ALU.mult, ALU.add)
    for pc in range(4):
        TS("dve", wif[:, :, pc], blk2, 512.0, float(pc * 128), ALU.mult, ALU.add)
    wif_flat = Buf(wif.ap.rearrange("p b c -> p (b c)"), wif.res)
    TS("dve", wif_flat, wif_flat, iota_p, None, ALU.add)
    CP("dve", WIDX, wif_flat)

    hsc = [sbt([D], BF16, "hsc%d" % i) for i in range(2)]
    for ti in range(NT):
        hb = hsc[ti % 2]
        DMA("sp", "hsc%d" % (ti % 2), hb, hnbuf[ti * 128:(ti + 1) * 128, :])
        SCATTER("sc%d" % (ti % 2), xg, hb, DEST1[:, ti:ti + 1])
        SCATTER("sc%d" % (ti % 2), xg, hb, DEST2[:, ti:ti + 1])

    xs = [sbt([D], BF16, "xs%d" % i) for i in range(2)]
    xT = [sbt([KC, 128], BF16, "xT%d" % i) for i in range(2)]
    NWB = 4
    wgp = [sbt([KC, 256], BF16, "wgp%d" % i) for i in range(NWB)]
    wup = [sbt([KC, 256], BF16, "wup%d" % i) for i in range(NWB)]
    wdp = [sbt([2, D], BF16, "wdp%d" % i) for i in range(NWB)]
    sg = sbt([4, 128], F32, "sg")
    hT = [sbt([2, 128], BF16, "hT%d" % i) for i in range(2)]
    yrow = [sbt([D], F32, "yrow%d" % i) for i in range(2)]
    print("phase B SBUF bytes/partition:", arena["off"])
    S.barrier()

    pieces = [(b, pc) for b in range(NB) for pc in range(4)]

    def load_piece(n):
        b, pc = pieces[n]
        slot = n % NWB
        idx = WIDX[:, b * 4 + pc:b * 4 + pc + 1]
        GATHER("wg%d" % slot, Buf(wgp[slot].ap.rearrange("p c f -> p (c f)"), wgp[slot].res), wg_r, idx, bound=NE * 512 - 1)
        GATHER("wu%d" % slot, Buf(wup[slot].ap.rearrange("p c f -> p (c f)"), wup[slot].res), wu_r, idx, bound=NE * 512 - 1)
        GATHER("wd%d" % slot, Buf(wdp[slot].ap.rearrange("p c f -> p (c f)"), wdp[slot].res), wd_r, idx, bound=NE * 512 - 1)

    ybanks = [pbank[0], pbank[1], pbank[2], pbank[3]]
    gub = [pbank[4], pbank[5]]
    NP = len(pieces)

    def stage_a(n):
        b, pc = pieces[n]
        xTb = xT[b % 2]
        if pc == 0:
            xsb = xs[b % 2]
            DMA("sp", "xs%d" % (b % 2), xsb, xg[b * 128:(b + 1) * 128, :])
            for half in range(2):
                pt = psb()
                for c in range(8):
                    k = half * 8 + c
                    TR(pt[:, c * 128:(c + 1) * 128], xsb[:, k * 128:(k + 1) * 128], identb)
                for c in range(8):
                    k = half * 8 + c
                    ACT(xTb[:, k, :], pt[:, c * 128:(c + 1) * 128], AF.Identity, bias=sh2T[:, k:k + 1], scale=g2T[:, k:k + 1])
        if n + 2 < NP:
            load_piece(n + 2)
        slot = n % NWB
        pgu = gub[n % 2]
        for fc in range(2):
            for gu, wsrc in ((0, wgp[slot]), (1, wup[slot])):
                q4 = fc * 2 + gu
                for k in range(KC):
                    MM(pgu[:, q4 * 128:(q4 + 1) * 128], wsrc[:, k, fc * 128:(fc + 1) * 128], xTb[:, k, :],
                       k == 0, k == KC - 1)

    def stage_b(n):
        pgu = gub[n % 2]
        htb = hT[n % 2]
        for fc in range(2):
            ACT(sg[:, fc, :], pgu[:, (fc * 2) * 128:(fc * 2 + 1) * 128], AF.Silu)
            TT("dve", htb[:, fc, :], sg[:, fc, :], pgu[:, (fc * 2 + 1) * 128:(fc * 2 + 2) * 128], ALU.mult)

    def stage_c(n):
        b, pc = pieces[n]
        slot = n % NWB
        htb = hT[n % 2]
        for dblk in range(4):
            for fc in range(2):
                MM(ybanks[dblk], htb[:, fc, :], wdp[slot][:, fc, dblk * 512:(dblk + 1) * 512],
                   pc == 0 and fc == 0, pc == 3 and fc == 1)
        if pc == 3:
            yr = yrow[b % 2]
            for dblk in range(4):
                if dblk % 2 == 0:
                    ACT(yr[:, dblk * 512:(dblk + 1) * 512], ybanks[dblk], AF.Identity)
                else:
                    CP("dve", yr[:, dblk * 512:(dblk + 1) * 512], ybanks[dblk])
            DMA("sp", "yst%d" % (b % 2), yslot[b * 128:(b + 1) * 128, :], yr, track_out=False)

    load_piece(0)
    load_piece(1)
    stage_a(0)
    for n in range(NP):
        if n + 1 < NP:
            stage_a(n + 1)
        stage_b(n)
        stage_c(n)

    S.barrier()
    arena["off"] = phase_base
    gate2_bc = sbt([D], F32, "gate2bc")
    nf_bc = sbt([D], F32, "nfbc")
    wstC = [sbt([KC, 256], BF16, "wstC%d" % i) for i in range(NWS)]
    badabC = sbt([256], F32, "badabC")
    csbC = sbt([KC, 128], BF16, "csbC")
    csTC = sbt([KC], F32, "csTC")
    h1c = [sbt([D], F32, "h1c%d" % i) for i in range(2)]
    y1c = [sbt([D], F32, "y1c%d" % i) for i in range(2)]
    y2c = [sbt([D], F32, "y2c%d" % i) for i in range(2)]
    acc = sbt([D], F32, "acc")
    outc = [sbt([D], F32, "outc%d" % i) for i in range(2)]
    junk = sbt([D], BF16, "junk")
    ssc = sbt([4], F32, "ssc")
    print("phase C SBUF bytes/partition:", arena["off"])
    S.barrier()
    wst[:] = wstC
    DMA("sp", "u14", nf_bc, bc(nfin_d, [128, D]))
    DMA("sp", "u15", csTC, cT_d)
    ACT(csTC, csTC, AF.Silu)
    for ch in range(KC):
        CP("dve", csbC[:, ch, :], bc(csTC[:, ch:ch + 1], [128, 128]))
    for j in range(8):
        def consC(wb, j=j):
            ps = psf()
            for ch in range(KC):
                MM(ps[:, 0:256], csbC[:, ch, :], wb[:, ch, 0:256], ch == 0, ch == KC - 1)
            DMA("sp", "bada", badabC, bc(b_ada[:, 5 * D + j * 256:5 * D + (j + 1) * 256], [128, 256]))
            TT("dve", gate2_bc[:, j * 256:(j + 1) * 256], ps[:, 0:256], badabC, ALU.add)
        wblock(w_ada, KC, 5 * D + j * 256, 256, consC)
    run_wq()
    last = []
    for ti in range(NT):
        r0 = ti * 128
        hb, y1, y2, ob = h1c[ti % 2], y1c[ti % 2], y2c[ti % 2], outc[ti % 2]
        DMA("sp", "h1c%d" % (ti % 2), hb, h1buf[r0:r0 + 128, :])
        GATHER("y1c%d" % (ti % 2), y1, yslot, DEST1[:, ti:ti + 1])
        GATHER("y2c%d" % (ti % 2), y2, yslot, DEST2[:, ti:ti + 1])
        TS("dve", acc, y1, W1[:, ti:ti + 1], None, ALU.mult)
        STT("dve", acc, y2, W2[:, ti:ti + 1], acc, ALU.mult, ALU.add)
        TT("dve", acc, acc, gate2_bc, ALU.mult)
        TT("dve", acc, acc, hb, ALU.add)
        ACT(junk, acc, AF.Square, accum=ssc[:, 0:1])
        ACT(ssc[:, 1:2], ssc[:, 0:1], AF.Ln, scale=1.0 / D, bias=EPS)
        ACT(ssc[:, 1:2], ssc[:, 1:2], AF.Exp, scale=-0.5)
        STT("dve", ob, acc, ssc[:, 1:2], nf_bc, ALU.mult, ALU.mult)
        last.append(DMA("sp", "oc%d" % (ti % 2), out_d[r0:r0 + 128, :], ob, track_out=False))
    S.emit(final_waits=last[-2:])
    return nc


def _consts():
    idx = np.arange(128)
    ident = np.eye(128, dtype=np.float32)
    causal = idx[:, None] <= idx[None, :]
    maskatt = causal.astype(np.float32)
    triS = maskatt * np.float32(-1.0 / 16.0)
    L = (idx[:, None] < idx[None, :]).astype(np.float32)
    ones = np.ones((128, 128), np.float32)
    cm = np.zeros((8, 128, 128), np.float32)
    cm[0], cm[1], cm[2], cm[3], cm[4] = ident, triS, maskatt, L, ones
    wins = (2, 4, 8, 16)
    acur = np.zeros((4, 128, 128), np.float32)
    aprev = np.zeros((4, 128, 128), np.float32)
    afirst = np.zeros((4, 128, 128), np.float32)
    s = idx[:, None]
    t = idx[None, :]
    for g, w in enumerate(wins):
        acur[g] = ((s <= t) & (s >= t - w + 1)).astype(np.float32) / w - ident
        aprev[g] = ((s - 128) >= (t - w + 1)).astype(np.float32) / w
        cnt = np.minimum(t + 1, w).astype(np.float32)
        afirst[g] = ((s <= t) & (s >= t - w + 1)).astype(np.float32) / cnt - ident
    iot = np.zeros((128, 73), np.float32)
    iot[:, 0:64] = np.arange(64)[None, :]
    iot[:, 64] = np.arange(128)
    iot[:, 65:73] = np.arange(8)[None, :]
    return cm, acur, aprev, afirst, iot


def _prep_shared(inp):
    f = lambda a: np.ascontiguousarray(np.asarray(a, dtype=np.float32))
    sh = {}
    sh["w_ada"] = f(inp["w_ada"][0])
    sh["b_ada"] = f(inp["b_ada"][0]).reshape(1, -1)
    sh["nmixT"] = f(inp["norm_mix"][0].reshape(KC, 128).T)
    sh["w_in"] = f(inp["w_in"][0])
    sh["w_gk2"] = f(inp["w_gk2"][0])
    sh["b_gk2"] = f(inp["b_gk2"][0]).reshape(1, -1)
    sh["gla_norm"] = f(inp["gla_norm"][0]).reshape(1, -1)
    sh["w_a"] = f(inp["w_a"][0])
    sh["w_pool"] = f(inp["w_pool"][0])
    sh["pscT"] = f(inp["pool_scale"][0].reshape(8, 128).T)
    sh["w_b"] = f(inp["w_b"][0])
    sh["w_out"] = f(inp["w_out"][0])
    sh["nffnT"] = f(inp["norm_ffn"][0].reshape(KC, 128).T)
    sh["w_r"] = f(np.concatenate([np.asarray(inp["w_rg"][0]), np.asarray(inp["w_re"][0])], axis=1))
    sh["b_r"] = f(np.concatenate([np.asarray(inp["b_rg"][0]), np.asarray(inp["b_re"][0])], axis=0)).reshape(1, -1)
    wg = np.asarray(inp["w_gate"][0], dtype=np.float32)
    wu = np.asarray(inp["w_up"][0], dtype=np.float32)
    wd = np.asarray(inp["w_down"][0], dtype=np.float32)
    sh["wg_r"] = np.ascontiguousarray(wg.reshape(NE, KC, 128, 4, 256).transpose(0, 3, 2, 1, 4)).reshape(NE * 512, 4096)
    sh["wu_r"] = np.ascontiguousarray(wu.reshape(NE, KC, 128, 4, 256).transpose(0, 3, 2, 1, 4)).reshape(NE * 512, 4096)
    sh["wd_r"] = np.ascontiguousarray(wd.reshape(NE, 4, 2, 128, D).transpose(0, 1, 3, 2, 4)).reshape(NE * 512, 4096)
    sh["nfin"] = f(inp["norm_final"]).reshape(1, -1)
    return sh


def run(inp, NST, dbg=False):
    x = np.asarray(inp["x"], dtype=np.float32)
    c = np.asarray(inp["c"], dtype=np.float32)
    B, SEQ, _ = x.shape
    T = NST * 512
    assert SEQ == 2 * T and B == 4
    nc = build_nc(NST, dbg)
    sh = _prep_shared(inp)
    cm, acur, aprev, afirst, iot = _consts()
    in_maps = []
    for core in range(8):
        b, half = core // 2, core % 2
        m = dict(sh)
        m["x_cur"] = np.ascontiguousarray(x[b, half * T:(half + 1) * T])
        m["x_prev"] = np.ascontiguousarray(x[b, 0:T])
        m["cT"] = np.ascontiguousarray(c[b].reshape(KC, 128).T)
        m["flag"] = np.full((128, 1), float(half), np.float32)
        m["cmat"] = cm
        m["apool"] = np.concatenate([acur, aprev, afirst if half == 0 else acur], axis=0)
        m["iotas"] = iot
        in_maps.append(m)
    res = run_bass_kernel_spmd(nc, in_maps, core_ids=list(range(8)))
    out = np.empty((B, SEQ, D), np.float32)
    for core in range(8):
        b, half = core // 2, core % 2
        out[b, half * T:(half + 1) * T] = res.results[core]["out"]
    return out, res


def kernel(**inputs):
    out, _ = run(inputs, 8)
    return out
```
